# Optimizing a Trainium2 kernel written in Bass

```python
import math
import jax, jax.numpy as jnp
from jax import lax
import numpy as np

D_MODEL = 1024
BATCH = 4
SEQ = 4096
DEPTH = 2

MEM_LEN = 256
RMS_EPS = 1e-6

A_HEADS = 8
A_HEAD_DIM = 64
A_WIDTH = A_HEADS * A_HEAD_DIM
IDX_HEADS = 8
IDX_DIM = 64
TOPK_MAX = 256
Q_BLOCK = 128

REL_BUCKETS = 32
REL_MAX_EXACT = 16
REL_MAX_DIST = 128

B_WIDTH = 512
CONV_K = 3

POOL_WINDOWS = (2, 4, 8, 16)
POOL_GROUPS = 4
POOL_GROUP = D_MODEL // POOL_GROUPS

X_HEADS = 4
X_HEAD_DIM = 128
X_WIDTH = X_HEADS * X_HEAD_DIM

D_FF = 2816

Q_END = A_WIDTH
K_END = 2 * A_WIDTH
V_END = 3 * A_WIDTH
IQ_END = V_END + IDX_HEADS * IDX_DIM
IK_END = IQ_END + IDX_DIM
IW_END = IK_END + IDX_HEADS
U_END = IW_END + B_WIDTH
GB_END = U_END + B_WIDTH
IN_COLS = GB_END + B_WIDTH
IN_SPLITS = (Q_END, K_END, V_END, IQ_END, IK_END, IW_END, U_END, GB_END)
MIX_WIDTH = A_WIDTH + B_WIDTH

N_EVEN = (DEPTH + 1) // 2
N_ODD = DEPTH // 2

kernel_name = "hybrid_dsa_shortconv_pool_macaron"


def rmsnorm(x, g):
    xf = x.astype(jnp.float32)
    y = xf * lax.rsqrt(jnp.mean(xf * xf, axis=-1, keepdims=True) + RMS_EPS)
    return (y * g.astype(jnp.float32)).astype(x.dtype)


def swiglu(h, w_gate, w_up, w_down):
    return (jax.nn.silu(h @ w_gate) * (h @ w_up)) @ w_down


def t5_bucket(dist):
    n = jnp.maximum(dist, 0)
    nf = jnp.maximum(n, 1).astype(jnp.float32)
    large = REL_MAX_EXACT + (jnp.log(nf / REL_MAX_EXACT)
                             / math.log(REL_MAX_DIST / REL_MAX_EXACT)
                             * (REL_BUCKETS - REL_MAX_EXACT)).astype(jnp.int32)
    large = jnp.minimum(large, REL_BUCKETS - 1)
    return jnp.where(n < REL_MAX_EXACT, n, large)


def dsa_attention(q, k, v, iq, ik, iw, rel_bias):
    b, s = q.shape[0], q.shape[1]
    n_sel = min(TOPK_MAX, s // 4)
    nblk = s // Q_BLOCK
    kf = k.reshape(b, s, A_WIDTH)
    vf = v.reshape(b, s, A_WIDTH)
    key_pos = jnp.arange(s, dtype=jnp.int32)
    iw = iw * (IDX_HEADS ** -0.5 * IDX_DIM ** -0.5)
    scale = A_HEAD_DIM ** -0.5

    def to_blocks(a):
        return jnp.moveaxis(a.reshape((b, nblk, Q_BLOCK) + a.shape[2:]), 1, 0)

    def block(args):
        qb, iqb, iwb, qpos = args
        dots = jnp.einsum('bqhd,bsd->bqhs', iqb, ik)
        idx_score = jnp.einsum('bqhs,bqh->bqs', jax.nn.relu(dots), iwb).astype(jnp.float32)
        causal = key_pos[None, :] <= qpos[:, None]
        idx_score = jnp.where(causal[None], idx_score, -jnp.inf)
        _, sel = lax.top_k(idx_score, n_sel)
        flat = sel.reshape(b, Q_BLOCK * n_sel)
        kg = jax.vmap(lambda a, i: a[i])(kf, flat).reshape(b, Q_BLOCK, n_sel, A_HEADS, A_HEAD_DIM)
        vg = jax.vmap(lambda a, i: a[i])(vf, flat).reshape(b, Q_BLOCK, n_sel, A_HEADS, A_HEAD_DIM)
        logits = jnp.einsum('bqhd,bqkhd->bqhk', qb, kg).astype(jnp.float32) * scale
        dist = qpos[None, :, None] - sel
        bias = rel_bias[t5_bucket(dist)].astype(jnp.float32)
        logits = logits + jnp.swapaxes(bias, -1, -2)
        logits = jnp.where((dist >= 0)[:, :, None, :], logits, -jnp.inf)
        p = jax.nn.softmax(logits, axis=-1).astype(vg.dtype)
        return jnp.einsum('bqhk,bqkhd->bqhd', p, vg)

    qpos_blocks = jnp.arange(s, dtype=jnp.int32).reshape(nblk, Q_BLOCK)
    out = lax.map(block, (to_blocks(q), to_blocks(iq), to_blocks(iw), qpos_blocks))
    return jnp.moveaxis(out, 0, 1).reshape(b, s, A_WIDTH)


def short_conv(u, w):
    s = u.shape[1]
    up = jnp.pad(u, ((0, 0), (CONV_K - 1, 0), (0, 0)))
    y = w[CONV_K - 1] * u
    for j in range(CONV_K - 1):
        y = y + w[j] * up[:, j:j + s]
    return y


def attn_conv_mixer(h, w_in, conv_w, w_out, rel_bias):
    b, s, _ = h.shape
    proj = h @ w_in
    q, k, v, iq, ik, iw, u, gb, gc = jnp.split(proj, IN_SPLITS, axis=-1)
    a = dsa_attention(q.reshape(b, s, A_HEADS, A_HEAD_DIM),
                      k.reshape(b, s, A_HEADS, A_HEAD_DIM),
                      v.reshape(b, s, A_HEADS, A_HEAD_DIM),
                      iq.reshape(b, s, IDX_HEADS, IDX_DIM), ik, iw, rel_bias)
    c = gb * short_conv(gc * u, conv_w)
    return jnp.concatenate([a, c], axis=-1) @ w_out


def pool_mixer(h, pool_w, pool_scale):
    b, s, d = h.shape
    cs = jnp.cumsum(h.astype(jnp.float32), axis=1)
    cs = jnp.pad(cs, ((0, 0), (1, 0), (0, 0)))
    t = jnp.arange(s)
    outs = []
    for g, win in enumerate(POOL_WINDOWS):
        sl = slice(g * POOL_GROUP, (g + 1) * POOL_GROUP)
        hi = cs[:, 1:, sl]
        lo = cs[:, jnp.maximum(t + 1 - win, 0), sl]
        cnt = jnp.minimum(t + 1, win).astype(jnp.float32)
        outs.append((hi - lo) / cnt[None, :, None])
    pooled = jnp.concatenate(outs, axis=-1).astype(h.dtype) - h
    y = jnp.einsum('bsgc,gcd->bsgd', pooled.reshape(b, s, POOL_GROUPS, POOL_GROUP), pool_w)
    return y.reshape(b, s, d) * pool_scale


def memory_cross_attention(h, mem_n, wq, wk, wv, wo):
    b, s, _ = h.shape
    m = mem_n.shape[1]
    q = (h @ wq).reshape(b, s, X_HEADS, X_HEAD_DIM)
    k = (mem_n @ wk).reshape(b, m, X_HEADS, X_HEAD_DIM)
    v = (mem_n @ wv).reshape(b, m, X_HEADS, X_HEAD_DIM)
    logits = jnp.einsum('bqhd,bkhd->bhqk', q, k).astype(jnp.float32) * (X_HEAD_DIM ** -0.5)
    p = jax.nn.softmax(logits, axis=-1).astype(v.dtype)
    o = jnp.einsum('bhqk,bkhd->bqhd', p, v).reshape(b, s, X_WIDTH)
    return o @ wo


def setup_inputs(seed: int = 0) -> dict:
    key = jax.random.key(seed)
    ks = jax.random.split(key, 24)
    nrm = jax.random.normal
    f32 = jnp.float32

    def gain(k, shape):
        return 1.0 + 0.05 * nrm(k, shape, f32)

    return {
        "x": nrm(ks[0], (BATCH, SEQ, D_MODEL), f32),
        "mem": nrm(ks[1], (BATCH, MEM_LEN, D_MODEL), f32),
        "ffn_w_gate": nrm(ks[2], (DEPTH, 2, D_MODEL, D_FF), f32) * D_MODEL ** -0.5,
        "ffn_w_up": nrm(ks[3], (DEPTH, 2, D_MODEL, D_FF), f32) * D_MODEL ** -0.5,
        "ffn_w_down": nrm(ks[4], (DEPTH, 2, D_FF, D_MODEL), f32) * D_FF ** -0.5,
        "ffn_norm_pre": gain(ks[5], (DEPTH, 2, D_MODEL)),
        "ffn_norm_post": gain(ks[6], (DEPTH, 2, D_MODEL)),
        "mix_norm_pre": gain(ks[7], (DEPTH, D_MODEL)),
        "mix_norm_post": gain(ks[8], (DEPTH, D_MODEL)),
        "even_w_in": nrm(ks[9], (N_EVEN, D_MODEL, IN_COLS), f32) * D_MODEL ** -0.5,
        "even_conv_w": nrm(ks[10], (N_EVEN, CONV_K, B_WIDTH), f32) * CONV_K ** -0.5,
        "even_w_out": nrm(ks[11], (N_EVEN, MIX_WIDTH, D_MODEL), f32) * MIX_WIDTH ** -0.5,
        "rel_bias": 0.5 * nrm(ks[12], (REL_BUCKETS, A_HEADS), f32),
        "pool_w": nrm(ks[13], (N_ODD, POOL_GROUPS, POOL_GROUP, POOL_GROUP), f32) * POOL_GROUP ** -0.5,
        "pool_scale": 1.0 + 0.1 * nrm(ks[14], (N_ODD, D_MODEL), f32),
        "xattn_norm_pre": gain(ks[15], (DEPTH, D_MODEL)),
        "xattn_mem_norm": gain(ks[16], (DEPTH, D_MODEL)),
        "xattn_norm_post": gain(ks[17], (DEPTH, D_MODEL)),
        "xattn_wq": nrm(ks[18], (DEPTH, D_MODEL, X_WIDTH), f32) * D_MODEL ** -0.5,
        "xattn_wk": nrm(ks[19], (DEPTH, D_MODEL, X_WIDTH), f32) * D_MODEL ** -0.5,
        "xattn_wv": nrm(ks[20], (DEPTH, D_MODEL, X_WIDTH), f32) * D_MODEL ** -0.5,
        "xattn_wo": nrm(ks[21], (DEPTH, X_WIDTH, D_MODEL), f32) * X_WIDTH ** -0.5,
    }


def reference(x, mem, ffn_w_gate, ffn_w_up, ffn_w_down, ffn_norm_pre, ffn_norm_post,
              mix_norm_pre, mix_norm_post, even_w_in, even_conv_w, even_w_out, rel_bias,
              pool_w, pool_scale, xattn_norm_pre, xattn_mem_norm, xattn_norm_post,
              xattn_wq, xattn_wk, xattn_wv, xattn_wo):
    for layer in range(DEPTH):
        h = rmsnorm(x, ffn_norm_pre[layer, 0])
        f = swiglu(h, ffn_w_gate[layer, 0], ffn_w_up[layer, 0], ffn_w_down[layer, 0])
        x = x + 0.5 * rmsnorm(f, ffn_norm_post[layer, 0])
        h = rmsnorm(x, mix_norm_pre[layer])
        if layer % 2 == 0:
            e = layer // 2
            y = attn_conv_mixer(h, even_w_in[e], even_conv_w[e], even_w_out[e], rel_bias)
        else:
            o = layer // 2
            y = pool_mixer(h, pool_w[o], pool_scale[o])
        x = x + rmsnorm(y, mix_norm_post[layer])
        h = rmsnorm(x, xattn_norm_pre[layer])
        mem_n = rmsnorm(mem, xattn_mem_norm[layer])
        c = memory_cross_attention(h, mem_n, xattn_wq[layer], xattn_wk[layer],
                                   xattn_wv[layer], xattn_wo[layer])
        x = x + rmsnorm(c, xattn_norm_post[layer])
        h = rmsnorm(x, ffn_norm_pre[layer, 1])
        f = swiglu(h, ffn_w_gate[layer, 1], ffn_w_up[layer, 1], ffn_w_down[layer, 1])
        x = x + 0.5 * rmsnorm(f, ffn_norm_post[layer, 1])
    return x
```

```python
import numpy as np
import concourse.bass as bass
import concourse.mybir as mybir
from concourse.bass_utils import run_bass_kernel_spmd

F32 = mybir.dt.float32
BF16 = mybir.dt.bfloat16
ALU = mybir.AluOpType
AF = mybir.ActivationFunctionType
AX = mybir.AxisListType

NT = 17
D = 1024
DFF = 2816
NCH = 22
EPS = 1e-6
NBIS = 18
STOP = None


class _Op:
    pass


class _Stop(Exception):
    pass


SUBSTOP = None


class Prog:
    def __init__(self, nc):
        self.nc = nc
        self.ops = []
        self.lw = {}
        self.rd = {}
        self.eng_last = {}
        self.pend_dma = []

    def add(self, eng, fn, r=(), w=(), dma=False, group=None):
        i = len(self.ops)
        op = _Op()
        op.eng, op.fn, op.dma, op.group, op.idx = eng, fn, dma, group, i
        op.deps = set()
        op.wkey = w[0] if (dma and len(w)) else None
        for k in r:
            p = self.lw.get(k)
            if p is not None:
                op.deps.add(p)
        for k in w:
            p = self.lw.get(k)
            if p is not None:
                op.deps.add(p)
            rr = self.rd.get(k)
            if rr:
                op.deps.update(rr['c'].values())
                op.deps.update(rr['d'])
        for k in r:
            rr = self.rd.setdefault(k, {'c': {}, 'd': []})
            if dma:
                rr['d'].append(i)
            else:
                rr['c'][eng] = i
        for k in w:
            self.lw[k] = i
            self.rd[k] = {'c': {}, 'd': []}
        op.deps.discard(i)
        if group is not None:
            for d_ in op.deps:
                assert self.ops[d_].group != group, ("intra-group dependency", group, r, w)
        self.ops.append(op)
        if dma:
            self.pend_dma.append(i)
        else:
            self.eng_last[eng] = i
        return i

    def barrier(self):
        prev = set(self.eng_last.values()) | set(self.pend_dma)
        self.pend_dma = []
        for eng in ('pe', 'act', 'dve', 'pool', 'sp'):
            i = len(self.ops)
            op = _Op()
            op.eng, op.fn, op.dma, op.group, op.idx = eng, None, False, None, i
            op.deps = set(prev)
            op.wkey = None
            self.ops.append(op)
            self.eng_last[eng] = i

    def emit(self):
        nc = self.nc
        ops = self.ops
        for op in ops:
            op.deps = {d for d in op.deps
                       if not (op.eng == 'pe' and ops[d].eng == 'pe' and not op.dma and not ops[d].dma
                               and op.fn is not None and ops[d].fn is not None)}
        needed = set()
        for op in ops:
            needed.update(op.deps)
        cnt = {}
        for op in ops:
            if op.dma:
                key = ('g', op.group) if op.group else ('k', op.wkey)
                cnt[key] = cnt.get(key, 0) + 16
                op.sig = (key, cnt[key])
            elif op.idx in needed and op.fn is not None:
                key = ('e', op.eng)
                cnt[key] = cnt.get(key, 0) + 1
                op.sig = (key, cnt[key])
            else:
                op.sig = None
        def targets(d, acc):
            o = ops[d]
            if o.fn is None:
                for dd in o.deps:
                    targets(dd, acc)
            else:
                key, val = o.sig
                if key[0] == 'g':
                    val = cnt[key]
                if val > acc.get(key, 0):
                    acc[key] = val
        sems = {}
        for key in cnt:
            sems[key] = nc.alloc_semaphore(name="s%d" % len(sems))
        self.nsem = len(sems)
        done_sem = nc.alloc_semaphore(name="sdone")
        bar_cache = {}
        with nc.Block() as block:
            decos = {'sp': block.sync, 'act': block.scalar, 'dve': block.vector,
                     'pool': block.gpsimd, 'pe': block.tensor}
            for name, deco in decos.items():
                def body(e, name=name):
                    known = {}
                    for op in ops:
                        if op.eng != name:
                            continue
                        acc = {}
                        for d in op.deps:
                            if ops[d].fn is None:
                                if d not in bar_cache:
                                    a2 = {}
                                    targets(d, a2)
                                    bar_cache[d] = a2
                                for k2, v2 in bar_cache[d].items():
                                    if v2 > acc.get(k2, 0):
                                        acc[k2] = v2
                            else:
                                targets(d, acc)
                        for key, val in acc.items():
                            if known.get(key, 0) < val:
                                e.wait_ge(sems[key], val)
                                known[key] = val
                        if op.fn is None:
                            continue
                        ins = op.fn(e)
                        if op.sig is not None:
                            ins.then_inc(sems[op.sig[0]], 16 if op.dma else 1)
                    if name != 'sp':
                        e.sem_inc(done_sem, 1)
                    else:
                        e.wait_ge(done_sem, 4)
                        for sm in sems.values():
                            e.sem_clear(sm)
                        e.sem_clear(done_sem)
                deco(body)


class Arena:
    def __init__(self, nc, base, limit):
        self.nc, self.top, self.limit = nc, base, limit
        self.n = 0

    def alloc(self, shape, dt):
        sz = int(np.prod(shape[1:])) * (4 if dt == F32 else 2)
        sz = (sz + 31) // 32 * 32
        assert self.top + sz <= self.limit, ("SBUF overflow", self.top, sz, self.limit)
        self.n += 1
        t = self.nc.alloc_sbuf_tensor_at("sb%d" % self.n, list(shape), dt, offset=self.top).ap()
        self.top += sz
        return t


def t5_bucket_np(d):
    n = np.maximum(d, 0)
    nf = np.maximum(n, 1).astype(np.float32)
    large = 16 + (np.log(nf / np.float32(16)) / np.float32(np.log(128 / 16)) * np.float32(16)).astype(np.int32)
    large = np.minimum(large, 31)
    return np.where(n < 16, n, large)


def build():
    nc = bass.Bass("TRN2", target_bir_lowering=False)
    P = Prog(nc)

    def din(name, shape):
        return nc.dram_tensor(name, list(shape), F32, kind="ExternalInput").ap()

    x_own = din("x_own", [2048, D])
    x_prev = din("x_prev", [2048, D])
    mem = din("mem", [256, D])
    w_gate = din("ffn_w_gate", [2, 2, D, DFF])
    w_up = din("ffn_w_up", [2, 2, D, DFF])
    w_down = din("ffn_w_down", [2, 2, DFF, D])
    ffn_pre = din("ffn_norm_pre", [2, 2, D])
    ffn_post = din("ffn_norm_post", [2, 2, D])
    mix_pre = din("mix_norm_pre", [2, D])
    mix_post = din("mix_norm_post", [2, D])
    w_in = din("even_w_in", [1, D, 3656])
    conv_w = din("even_conv_w", [1, 3, 512])
    w_out = din("even_w_out", [1, D, D])
    rel_bias = din("rel_bias", [32, 8])
    pool_w = din("pool_w", [1, 4, 256, 256])
    pool_scale = din("pool_scale", [1, D])
    xa_pre = din("xattn_norm_pre", [2, D])
    xa_mem = din("xattn_mem_norm", [2, D])
    xa_post = din("xattn_norm_post", [2, D])
    xa_wq = din("xattn_wq", [2, D, 512])
    xa_wk = din("xattn_wk", [2, D, 512])
    xa_wv = din("xattn_wv", [2, D, 512])
    xa_wo = din("xattn_wo", [2, 512, D])
    c_ident = din("c_ident", [128, 128])
    c_tri = din("c_tri", [128, 128])
    c_kb = din("c_kb", [1, 2048])
    c_onehot = din("c_onehot", [32, 383])
    c_halo = din("c_halo", [128, 1])
    c_corr = din("c_corr", [128, 64])
    c_cj = din("c_cj", [128, NBIS])
    out = nc.dram_tensor("out", [2048, D], F32, kind="ExternalOutput").ap()

    def dscr(name, shape, dt=BF16):
        return nc.dram_tensor(name, list(shape), dt).ap()

    wg_bf = [[dscr("wg_bf%d%d" % (l, i), [D, DFF]) for i in range(2)] for l in range(2)]
    wu_bf = [[dscr("wu_bf%d%d" % (l, i), [D, DFF]) for i in range(2)] for l in range(2)]
    wd_bf = [[dscr("wd_bf%d%d" % (l, i), [DFF, D]) for i in range(2)] for l in range(2)]
    win_bf = dscr("win_bf", [D, 3656])
    wout_bf = dscr("wout_bf", [D, D])
    poolw_bf = dscr("poolw_bf", [4, 256, 256])
    xq_bf = [dscr("xq_bf%d" % l, [D, 512]) for l in range(2)]
    xk_bf = [dscr("xk_bf%d" % l, [D, 512]) for l in range(2)]
    xv_bf = [dscr("xv_bf%d" % l, [D, 512]) for l in range(2)]
    xo_bf = [dscr("xo_bf%d" % l, [512, D]) for l in range(2)]
    xspill = dscr("xspill", [NT, 128, D], F32)
    kT_prev = dscr("kT_prev", [128, 4, 2048])
    v_prev = dscr("v_prev", [128, 16, 512])
    ik_prev = dscr("ik_prev", [128, 2048])
    tv_d = dscr("tv_d", [8, 383], F32)

    import os
    SKIP = os.environ.get("KSKIP", "").split(",")

    def cast(dst, src, key):
        if "casts" in SKIP:
            return
        P.add('pool', lambda e: e.dma_start(out=dst, in_=src, max_dma_last_dim=4096), r=(), w=(key,), dma=True)

    cast_order = []

    def cast_ffn(l, i):
        cast(wg_bf[l][i], w_gate[l, i], ("wg", l, i))
        cast(wu_bf[l][i], w_up[l, i], ("wu", l, i))
        cast(wd_bf[l][i], w_down[l, i], ("wd", l, i))

    def cast_xa(l):
        cast(xq_bf[l], xa_wq[l], ("xq", l))
        cast(xk_bf[l], xa_wk[l], ("xk", l))
        cast(xv_bf[l], xa_wv[l], ("xv", l))
        cast(xo_bf[l], xa_wo[l], ("xo", l))

    ar = Arena(nc, 16512, 229344)
    identb = ar.alloc([128, 128], BF16)
    identf = ar.alloc([128, 128], F32)
    onesb = ar.alloc([128, 128], BF16)
    trif = ar.alloc([128, 128], F32)
    cjtab = ar.alloc([128, NBIS], F32)
    kbrow = ar.alloc([1, 2048], BF16)
    gT = ar.alloc([128, 12, 8], F32)
    st = ar.alloc([128, 64], F32)
    negh = ar.alloc([128, 1], F32)
    cw = ar.alloc([128, 4, 3], F32)
    cfar = ar.alloc([128, 8], F32)
    halof = ar.alloc([128, 1], F32)
    corr = ar.alloc([128, 64], F32)
    gpost = ar.alloc([128, D], F32)
    X = ar.alloc([128, NT, D], F32)
    X_base = ar.top - NT * D * 4
    phase_base = ar.top

    ps = [nc.alloc_psum_tensor("psb%d" % i, [128, 512], F32).ap() for i in range(8)]

    def psbf(i):
        return ps[i].bitcast(BF16)

    def ld(dst, src, key, eng='sp', **kw):
        P.add(eng, lambda e: e.dma_start(out=dst, in_=src, **kw), r=(), w=(key,), dma=True, group=("consts" if eng == 'sp' else None))

    ld(identf, c_ident, "identf")
    ld(identb, c_ident, "identb", eng='pool')
    ld(trif, c_tri, "trif")
    ld(cjtab, c_cj, "cjtab")
    if "kb" not in SKIP:
        ld(kbrow, c_kb, "kbrow", eng='pool')
    ld(halof, c_halo, "halof")
    ld(corr, c_corr, "corr")
    if "cfar" not in SKIP:
      ld(cfar, rel_bias[31:32, :].partition_broadcast(128) if False else rel_bias[31, :].partition_broadcast(128), "cfar")
    norm_vecs = [ffn_pre[0, 0], ffn_pre[0, 1], ffn_pre[1, 0], ffn_pre[1, 1], mix_pre[0], mix_pre[1],
                 xa_pre[0], xa_pre[1], xa_mem[0], xa_mem[1]]
    G_FFN = {(0, 0): 0, (0, 1): 1, (1, 0): 2, (1, 1): 3}
    G_MIX = {0: 4, 1: 5}
    G_XA = {0: 6, 1: 7}
    G_MEM = {0: 8, 1: 9}
    for n, v in enumerate(norm_vecs):
        def f(e, n=n, v=v):
            with nc.allow_non_contiguous_dma(reason="tiny gain vector transpose"):
                return e.dma_start(out=gT[:, n, :], in_=v.rearrange("(k p) -> p k", p=128))
        P.add('sp', f, r=(), w=(("gT", n),), dma=True, group="consts")

    for j in range(3 if "cw" not in SKIP else 0):
        for c4 in range(4):
            def f(e, j=j, c4=c4):
                with nc.allow_non_contiguous_dma(reason="tiny conv weights"):
                    return e.dma_start(out=cw[:, c4, j:j + 1], in_=conv_w[0, j, c4 * 128:(c4 + 1) * 128].rearrange("(p o) -> p o", o=1))
            P.add('sp', f, r=(), w=(("cw", j * 4 + c4),), dma=True, group="consts")
    P.add('pool', lambda e: e.memset(negh, -0.5), w=("negh",))
    P.add('pool', lambda e: e.memset(onesb, 1.0), w=("onesb",))
    CONSTS = ("identf", "identb", "trib", "kbrow", "halof", "corr", "cfar", "cw", "negh", "onesb") + tuple(("gT", n) for n in range(10))

    cast_ffn(0, 0)
    cast(win_bf, w_in[0], ("win",))
    cast(wout_bf, w_out[0], ("wout",))
    cast_xa(0)
    cast_ffn(0, 1)
    cast_ffn(1, 0)
    cast(poolw_bf, pool_w[0], ("poolw",))
    cast_xa(1)
    cast_ffn(1, 1)

    stc = [0]

    def stslot(n=1):
        s = stc[0]
        stc[0] = (stc[0] + n) % 60
        if s + n > 60:
            s = 0
            stc[0] = n
        return s

    def rstd_from_ss(ss_ap, ss_key, out_ap, out_key, mul=1.0):
        m2 = mul * mul
        P.add('pool', lambda e: e.tensor_scalar(out=out_ap, in0=ss_ap, scalar1=1.0 / (D * m2), scalar2=EPS / m2, op0=ALU.mult, op1=ALU.add),
              r=(ss_key,), w=(out_key,))
        P.add('pool', lambda e: e.tensor_tensor(out=out_ap, in0=out_ap, in1=negh, op=ALU.pow), r=(out_key, "negh"), w=(out_key,))

    class NS:
        pass

    def alloc_ns(a, nt=2):
        ns = NS()
        ns.xn = [a.alloc([128, D], BF16) for _ in range(2)]
        ns.sqj = a.alloc([128, D], BF16)
        ns.t = [a.alloc([128, D], F32) for _ in range(nt)]
        ns.nt = nt
        ns.c = 0
        return ns

    def norm_transpose(ns, src_ap, src_key, gidx, dst_fn, dst_key, tpbank):
        s = stslot(2)
        b = ns.c % 2
        ns.c += 1
        xn = ns.xn[b]
        P.add('act', lambda e: e.activation(out=ns.sqj, in_=src_ap, func=AF.Square, accum_out=st[:, s:s + 1]),
              r=(src_key,), w=("sqj", ("st", s)))
        rstd_from_ss(st[:, s:s + 1], ("st", s), st[:, s + 1:s + 2], ("st", s + 1))
        P.add('dve', lambda e: e.tensor_scalar(out=xn, in0=src_ap, scalar1=st[:, s + 1:s + 2], scalar2=None, op0=ALU.mult),
              r=(src_key, ("st", s + 1)), w=(("xn", b),))
        pb = psbf(tpbank)

        def tp(e):
            for kc in range(8):
                ins = e.transpose(pb[:, kc * 128:(kc + 1) * 128], xn[:, kc * 128:(kc + 1) * 128], identb)
            return ins
        P.add('pe', tp, r=(("xn", b), "identb"), w=(("ps", tpbank),))

        def ev(e):
            for kc in range(8):
                ins = e.tensor_scalar(out=dst_fn(kc), in0=pb[:, kc * 128:(kc + 1) * 128], scalar1=gT[:, gidx, kc:kc + 1],
                                      scalar2=None, op0=ALU.mult)
            return ins
        P.add('dve', ev, r=(("ps", tpbank), ("gT", gidx)), w=(dst_key,))

    def load_gpost(vec):
        P.add('sp', lambda e: e.dma_start(out=gpost, in_=vec.partition_broadcast(128)), r=(), w=("gpost",), dma=True)

    def epilogue(ns, src_halves, src_keys, t, factor, psc=None):
        s = stslot(4)
        b = ns.c % ns.nt
        ns.c += 1
        tt = ns.t[b]
        if psc is not None:
            for hh in range(2):
                P.add('dve', lambda e, hh=hh: e.tensor_tensor(out=tt[:, hh * 512:(hh + 1) * 512], in0=src_halves[hh],
                                                              in1=psc[:, hh * 512:(hh + 1) * 512], op=ALU.mult),
                      r=(src_keys[hh], "psc"), w=(("t", b),))
            srcs = [tt[:, 0:512], tt[:, 512:1024]]
            skeys = [("t", b), ("t", b)]
        else:
            srcs, skeys = src_halves, src_keys
        for hh in range(2):
            P.add('act', lambda e, hh=hh: e.activation(out=ns.sqj[:, hh * 512:(hh + 1) * 512], in_=srcs[hh], func=AF.Square,
                                                        accum_out=st[:, s + hh:s + hh + 1]),
                  r=(skeys[hh],), w=("sqj", ("st", s + hh)))
        P.add('dve', lambda e: e.tensor_tensor(out=st[:, s + 2:s + 3], in0=st[:, s:s + 1], in1=st[:, s + 1:s + 2], op=ALU.add),
              r=(("st", s), ("st", s + 1)), w=(("st", s + 2),))
        rstd_from_ss(st[:, s + 2:s + 3], ("st", s + 2), st[:, s + 3:s + 4], ("st", s + 3), mul=factor)
        for hh in range(2):
            P.add('dve', lambda e, hh=hh: e.tensor_tensor(out=tt[:, hh * 512:(hh + 1) * 512], in0=srcs[hh],
                                                          in1=gpost[:, hh * 512:(hh + 1) * 512], op=ALU.mult),
                  r=(skeys[hh], "gpost"), w=(("t", b),))
        P.add('dve', lambda e: e.scalar_tensor_tensor(out=X[:, t, :], in0=tt, scalar=st[:, s + 3:s + 4], in1=X[:, t, :],
                                                      op0=ALU.mult, op1=ALU.add),
              r=(("t", b), ("st", s + 3), ("X", t)), w=(("X", t),))

    def ffn_phase(l, i, groups, first_src=None, after_group=None):
        a = Arena(nc, phase_base, ar.limit)
        Wd = a.alloc([128, NCH, D], BF16)
        actT = a.alloc([128, NCH, 512], BF16)
        hT = a.alloc([128, 8, 512], BF16)
        wgu = [a.alloc([128, 2, 8, 256], BF16) for _ in range(2)]
        sg = [a.alloc([128, 512], F32) for _ in range(2)]
        ns = alloc_ns(a)
        wdv = wd_bf[l][i].rearrange("(c p) d -> p c d", p=128)
        for q in range(2):
            P.add('sp', lambda e, q=q: e.dma_start(out=Wd[:, q * 11:(q + 1) * 11, :], in_=wdv[:, q * 11:(q + 1) * 11, :]),
                  r=(("wd", l, i),), w=(("Wd", q),), dma=True)
        load_gpost(ffn_post[l, i])
        wgv = wg_bf[l][i].rearrange("(kc p) f -> p kc f", p=128)
        wuv = wu_bf[l][i].rearrange("(kc p) f -> p kc f", p=128)
        gidx = G_FFN[(l, i)]
        sc_count = [0]
        for gi, (t0, t1) in enumerate(groups):
            n = t1 - t0
            T = n * 128
            for j in range(n):
                norm_transpose(ns, X[:, t0 + j, :], ("X", t0 + j), gidx,
                               lambda kc, j=j: hT[:, kc, j * 128:(j + 1) * 128], "hT", 7)
            if SUBSTOP == "nt":
                raise _Stop()
            for sc in range(11):
                slot = sc_count[0] % 2
                sc_count[0] += 1
                P.add('sp', lambda e, sc=sc, slot=slot: e.dma_start(out=wgu[slot][:, 0], in_=wgv[:, :, sc * 256:(sc + 1) * 256]),
                      r=(("wg", l, i),), w=(("wgu", slot, 0),), dma=True)
                P.add('sp', lambda e, sc=sc, slot=slot: e.dma_start(out=wgu[slot][:, 1], in_=wuv[:, :, sc * 256:(sc + 1) * 256]),
                      r=(("wu", l, i),), w=(("wgu", slot, 1),), dma=True)
                for hh in range(2):
                    c = sc * 2 + hh
                    bk = c % 2
                    for which in range(2):
                        bank = which * 2 + bk

                        def mm(e, which=which, bank=bank, slot=slot, hh=hh):
                            for kc in range(8):
                                ins = e.matmul(ps[bank][:, :T], wgu[slot][:, which, kc, hh * 128:(hh + 1) * 128], hT[:, kc, :T],
                                               start=(kc == 0), stop=(kc == 7))
                            return ins
                        P.add('pe', mm, r=(("wgu", slot, which), "hT"), w=(("ps", bank),))
                    P.add('act', lambda e, bk=bk: e.activation(out=sg[bk][:, :T], in_=ps[bk][:, :T], func=AF.Silu),
                          r=(("ps", bk),), w=(("sg", bk),))
                    P.add('dve', lambda e, bk=bk, c=c: e.tensor_tensor(out=actT[:, c, :T], in0=ps[2 + bk][:, :T], in1=sg[bk][:, :T], op=ALU.mult),
                          r=(("ps", 2 + bk), ("sg", bk)), w=(("actT", c),))
            if SUBSTOP == "pa":
                raise _Stop()
            for j in range(n):
                banks = (4, 5)
                for dh in range(2):
                    def mm(e, j=j, dh=dh, bank=banks[dh]):
                        for c in range(NCH):
                            ins = e.matmul(ps[bank], actT[:, c, j * 128:(j + 1) * 128], Wd[:, c, dh * 512:(dh + 1) * 512],
                                           start=(c == 0), stop=(c == NCH - 1))
                        return ins
                    P.add('pe', mm, r=tuple(("actT", c) for c in range(NCH)) + (("Wd", 0), ("Wd", 1)), w=(("ps", banks[dh]),))
                epilogue(ns, [ps[banks[0]], ps[banks[1]]], [("ps", banks[0]), ("ps", banks[1])], t0 + j, 0.5)
            if SUBSTOP == "pb":
                raise _Stop()
            if after_group is not None:
                after_group(gi, a, ns, hT, actT)

    xo_v = x_own.rearrange("(t p) d -> p t d", p=128)
    xp_v = x_prev.rearrange("(t p) d -> p t d", p=128)

    def prev_pass():
        pass

    GROUPS17 = [(0, 1), (1, 5), (5, 9), (9, 13), (13, 17)]
    GROUPS16 = [(1, 5), (5, 9), (9, 13), (13, 17)]

    def prev_ffn():
        PG = [(13, 17)] * 4
        winv = win_bf.rearrange("(kc p) f -> p kc f", p=128)

        def pre_load(gi):
            for j in range(4):
                P.add('sp', lambda e, j=j, gi=gi: e.dma_start(out=X[:, 13 + j, :], in_=xp_v[:, gi * 4 + j, :]),
                      r=(), w=(("X", 13 + j),), dma=True, group=("xprev", gi))

        a_holder = {}

        def after(gi, a, nsl, hTp, actT):
            if 'wk' not in a_holder:
                a_holder['wk'] = a.alloc([128, 8, 512], BF16)
                a_holder['wv'] = a.alloc([128, 8, 512], BF16)
                a_holder['wik'] = a.alloc([128, 8, 128], BF16)
                P.add('sp', lambda e: e.dma_start(out=a_holder['wk'], in_=winv[:, :, 512:1024]), r=(("win",),), w=("pwk",), dma=True)
                P.add('sp', lambda e: e.dma_start(out=a_holder['wv'], in_=winv[:, :, 1024:1536]), r=(("win",),), w=("pwv",), dma=True)
                P.add('sp', lambda e: e.dma_start(out=a_holder['wik'][:, :, 0:64], in_=winv[:, :, 2048:2112]), r=(("win",),), w=("pwik0",), dma=True)
                P.add('sp', lambda e: e.dma_start(out=a_holder['wik'][:, :, 64:128], in_=winv[:, :, 2048:2112]), r=(("win",),), w=("pwik1",), dma=True)
            wk, wv, wik = a_holder['wk'], a_holder['wv'], a_holder['wik']
            ko, vo, iko = actT[:, 0:4, :], actT[:, 4:8, :], actT[:, 8, :]
            KO = tuple(("actT", c) for c in range(0, 4))
            VO = tuple(("actT", c) for c in range(4, 8))
            IKO = (("actT", 8),)
            for j in range(4):
                norm_transpose(nsl, X[:, 13 + j, :], ("X", 13 + j), G_MIX[0],
                               lambda kc, j=j: hTp[:, kc, j * 128:(j + 1) * 128], "hT", 7)
            for ci in range(4):
                bank = ci % 2
                def mm(e, ci=ci, bank=bank):
                    for kc in range(8):
                        ins = e.matmul(ps[bank], wk[:, kc, ci * 128:(ci + 1) * 128], hTp[:, kc, :], start=(kc == 0), stop=(kc == 7))
                    return ins
                P.add('pe', mm, r=("pwk", "hT"), w=(("ps", bank),))
                P.add('act', lambda e, ci=ci, bank=bank: e.activation(out=ko[:, ci, :], in_=ps[bank], func=AF.Copy), r=(("ps", bank),), w=(("actT", ci),))
            def mm(e):
                for kc in range(8):
                    ins = e.matmul(ps[2], wik[:, kc, :], hTp[:, kc, :], start=(kc == 0), stop=(kc == 7))
                return ins
            P.add('pe', mm, r=("pwik0", "pwik1", "hT"), w=(("ps", 2),))
            P.add('act', lambda e: e.activation(out=iko, in_=ps[2], func=AF.Copy), r=(("ps", 2),), w=IKO)
            for j in range(4):
                bank = 4 + j % 2
                def mm(e, j=j, bank=bank):
                    for kc in range(8):
                        ins = e.matmul(ps[bank], hTp[:, kc, j * 128:(j + 1) * 128], wv[:, kc, :], start=(kc == 0), stop=(kc == 7))
                    return ins
                P.add('pe', mm, r=("pwv", "hT"), w=(("ps", bank),))
                P.add('act', lambda e, j=j, bank=bank: e.activation(out=vo[:, j, :], in_=ps[bank], func=AF.Copy), r=(("ps", bank),), w=(("actT", 4 + j),))
            P.add('sp', lambda e, gi=gi: e.dma_start(out=kT_prev[:, :, gi * 512:(gi + 1) * 512], in_=ko), r=KO, w=("kTp",), dma=True)
            P.add('sp', lambda e, gi=gi: e.dma_start(out=v_prev[:, gi * 4:(gi + 1) * 4, :], in_=vo), r=VO, w=("vp",), dma=True)
            P.add('sp', lambda e, gi=gi: e.dma_start(out=ik_prev[:, gi * 512:(gi + 1) * 512], in_=iko), r=IKO, w=("ikp",), dma=True)
            if gi < 3:
                pre_load(gi + 1)
            else:
                P.add('pool', lambda e: e.tensor_copy(out=X[:, 0, :], in_=X[:, 16, :]), r=(("X", 16),), w=(("X", 0),))
        return PG, pre_load, after, a_holder

    stage = {}

    def stop_here(name):
        return STOP == name


    def xattn_phase(l, groups):
        a = Arena(nc, phase_base, ar.limit)
        memx = a.alloc([128, 2, D], F32)
        memT = a.alloc([128, 8, 256], BF16)
        kxT = a.alloc([128, 4, 256], BF16)
        vx = a.alloc([128, 2, 512], BF16)
        Wq = a.alloc([128, 8, 512], BF16)
        Wk = a.alloc([128, 8, 512], BF16)
        Wv = a.alloc([128, 8, 512], BF16)
        Wo = a.alloc([128, 4, D], BF16)
        hT = a.alloc([128, 8, 512], BF16)
        qxT = a.alloc([128, 4, 512], BF16)
        PTx = [a.alloc([128, 2, 512], BF16) for _ in range(2)]
        rs = a.alloc([128, 512], F32)
        oTn = a.alloc([128, 4, 512], BF16)
        ns = alloc_ns(a)
        P.add('sp', lambda e: e.dma_start(out=Wq, in_=xq_bf[l].rearrange("(kc p) f -> p kc f", p=128)), r=(("xq", l),), w=("xWq",), dma=True)
        P.add('sp', lambda e: e.dma_start(out=Wk, in_=xk_bf[l].rearrange("(kc p) f -> p kc f", p=128)), r=(("xk", l),), w=("xWk",), dma=True)
        P.add('sp', lambda e: e.dma_start(out=Wv, in_=xv_bf[l].rearrange("(kc p) f -> p kc f", p=128)), r=(("xv", l),), w=("xWv",), dma=True)
        P.add('sp', lambda e: e.dma_start(out=Wo, in_=xo_bf[l].rearrange("(h p) d -> p h d", p=128)), r=(("xo", l),), w=("xWo",), dma=True)
        P.add('sp', lambda e: e.dma_start(out=memx, in_=mem.rearrange("(t p) d -> p t d", p=128)), r=(), w=("memx",), dma=True)
        load_gpost(xa_post[l])
        for mt in range(2):
            norm_transpose(ns, memx[:, mt, :], "memx", G_MEM[l], lambda kc, mt=mt: memT[:, kc, mt * 128:(mt + 1) * 128], "memT", 7)
        for h in range(4):
            bank = h % 2
            def mm(e, h=h, bank=bank):
                for kc in range(8):
                    ins = e.matmul(ps[bank][:, :256], Wk[:, kc, h * 128:(h + 1) * 128], memT[:, kc, :], start=(kc == 0), stop=(kc == 7))
                return ins
            P.add('pe', mm, r=("xWk", "memT"), w=(("ps", bank),))
            P.add('act', lambda e, h=h, bank=bank: e.activation(out=kxT[:, h, :], in_=ps[bank][:, :256], func=AF.Copy), r=(("ps", bank),), w=("kxT",))
        for mt in range(2):
            bank = 2 + mt
            def mm(e, mt=mt, bank=bank):
                for kc in range(8):
                    ins = e.matmul(ps[bank], memT[:, kc, mt * 128:(mt + 1) * 128], Wv[:, kc, :], start=(kc == 0), stop=(kc == 7))
                return ins
            P.add('pe', mm, r=("xWv", "memT"), w=(("ps", bank),))
            P.add('act', lambda e, mt=mt, bank=bank: e.activation(out=vx[:, mt, :], in_=ps[bank], func=AF.Copy), r=(("ps", bank),), w=("vx",))
        cnt = [0]
        for (t0, t1) in groups:
            n = t1 - t0
            T = n * 128
            for j in range(n):
                norm_transpose(ns, X[:, t0 + j, :], ("X", t0 + j), G_XA[l], lambda kc, j=j: hT[:, kc, j * 128:(j + 1) * 128], "hT", 7)
            for h in range(4):
                pb = cnt[0] % 2
                cnt[0] += 1
                def mm(e, h=h):
                    for kc in range(8):
                        ins = e.matmul(ps[0][:, :T], Wq[:, kc, h * 128:(h + 1) * 128], hT[:, kc, :T], start=(kc == 0), stop=(kc == 7))
                    return ins
                P.add('pe', mm, r=("xWq", "hT"), w=(("ps", 0),))
                P.add('act', lambda e, h=h: e.activation(out=qxT[:, h, :T], in_=ps[0][:, :T], func=AF.Copy, scale=float(128 ** -0.5)),
                      r=(("ps", 0),), w=(("qxT", h),))
                for mt in range(2):
                    P.add('pe', lambda e, h=h, mt=mt: e.matmul(ps[1 + mt][:, :T], kxT[:, h, mt * 128:(mt + 1) * 128], qxT[:, h, :T], start=True, stop=True),
                          r=("kxT", ("qxT", h)), w=(("ps", 1 + mt),))
                    P.add('act', lambda e, mt=mt, pb=pb: e.activation(out=PTx[pb][:, mt, :T], in_=ps[1 + mt][:, :T], func=AF.Exp),
                          r=(("ps", 1 + mt),), w=(("PTx", pb, mt),))
                def mm(e, pb=pb):
                    for mt in range(2):
                        ins = e.matmul(ps[3][:, :T], onesb, PTx[pb][:, mt, :T], start=(mt == 0), stop=(mt == 1))
                    return ins
                P.add('pe', mm, r=("onesb", ("PTx", pb, 0), ("PTx", pb, 1)), w=(("ps", 3),))
                def mm(e, pb=pb, h=h):
                    for mt in range(2):
                        ins = e.matmul(ps[4][:, :T], vx[:, mt, h * 128:(h + 1) * 128], PTx[pb][:, mt, :T], start=(mt == 0), stop=(mt == 1))
                    return ins
                P.add('pe', mm, r=("vx", ("PTx", pb, 0), ("PTx", pb, 1)), w=(("ps", 4),))
                P.add('dve', lambda e: e.reciprocal(out=rs[:, :T], in_=ps[3][:, :T]), r=(("ps", 3),), w=("rs",))
                P.add('dve', lambda e, h=h: e.tensor_tensor(out=oTn[:, h, :T], in0=ps[4][:, :T], in1=rs[:, :T], op=ALU.mult),
                      r=(("ps", 4), "rs"), w=(("oTn", h),))
            for j in range(n):
                for dh in range(2):
                    def mm(e, j=j, dh=dh):
                        for h in range(4):
                            ins = e.matmul(ps[5 + dh], oTn[:, h, j * 128:(j + 1) * 128], Wo[:, h, dh * 512:(dh + 1) * 512], start=(h == 0), stop=(h == 3))
                        return ins
                    P.add('pe', mm, r=tuple(("oTn", h) for h in range(4)) + ("xWo",), w=(("ps", 5 + dh),))
                epilogue(ns, [ps[5], ps[6]], [("ps", 5), ("ps", 6)], t0 + j, 1.0)

    def pool_phase():
        a = Arena(nc, phase_base, ar.limit)
        L = 16 + NT * 128
        hTf = [a.alloc([128, L], F32) for _ in range(2)]
        sAB = [a.alloc([128, L], F32) for _ in range(2)]
        pooledT = a.alloc([128, 8, NT * 128], BF16)
        pw = a.alloc([128, 4, 2, 256], BF16)
        psc = a.alloc([128, D], F32)
        rstd17 = a.alloc([128, NT], F32)
        xs = [a.alloc([128, 128], F32) for _ in range(2)]
        ns = alloc_ns(a)
        for g in range(4):
            P.add('sp', lambda e, g=g: e.dma_start(out=pw[:, g], in_=poolw_bf[g].rearrange("(k p) d -> p k d", p=128)),
                  r=(("poolw",),), w=(("pw", g),), dma=True)
        P.add('sp', lambda e: e.dma_start(out=psc, in_=pool_scale[0].partition_broadcast(128)), r=(), w=("psc",), dma=True)
        load_gpost(mix_post[1])
        for t in range(NT):
            s_ = stslot(1)
            P.add('act', lambda e, t=t, s_=s_: e.activation(out=ns.sqj, in_=X[:, t, :], func=AF.Square, accum_out=st[:, s_:s_ + 1]),
                  r=(("X", t),), w=("sqj", ("st", s_)))
            rstd_from_ss(st[:, s_:s_ + 1], ("st", s_), rstd17[:, t:t + 1], ("rstd17", t))
        for i2 in range(2):
            P.add('dve', lambda e, i2=i2: e.memset(hTf[i2][:, 0:16], 0.0), w=(("hTf", i2),))
            P.add('dve', lambda e, i2=i2: e.memset(sAB[i2][:, 0:16], 0.0), w=(("sAB", i2),))
        xc = [0]
        for kc in range(8):
            g = kc // 2
            win = (2, 4, 8, 16)[g]
            hi = kc % 2
            hb = hTf[hi]
            for t in range(NT):
                b = xc[0] % 2
                xc[0] += 1
                bank = (t // 4) % 2
                P.add('dve', lambda e, t=t, b=b, kc=kc: e.tensor_scalar(out=xs[b], in0=X[:, t, kc * 128:(kc + 1) * 128], scalar1=rstd17[:, t:t + 1],
                                                                       scalar2=None, op0=ALU.mult),
                      r=(("X", t), ("rstd17", t)), w=(("xs", b),))
                P.add('pe', lambda e, t=t, b=b, bank=bank: e.transpose(ps[bank][:, (t % 4) * 128:(t % 4 + 1) * 128], xs[b], identf),
                      r=(("xs", b), "identf"), w=(("ps", bank),))
                if t % 4 == 3 or t == NT - 1:
                    tb = (t // 4) * 4
                    wd_ = (t - tb + 1) * 128
                    P.add('dve', lambda e, tb=tb, wd_=wd_, bank=bank, hb=hb, kc=kc: e.tensor_scalar(
                        out=hb[:, 16 + tb * 128:16 + tb * 128 + wd_], in0=ps[bank][:, :wd_], scalar1=gT[:, G_MIX[1], kc:kc + 1], scalar2=None, op0=ALU.mult),
                        r=(("ps", bank), ("gT", G_MIX[1])), w=(("hTf", hi),))
            P.add('dve', lambda e, hb=hb: e.tensor_scalar(out=hb[:, 16:144], in0=hb[:, 16:144], scalar1=halof[:, 0:1], scalar2=None, op0=ALU.mult),
                  r=(("hTf", hi), "halof"), w=(("hTf", hi),))
            cur, curk = hb, ("hTf", hi)
            sh = 1
            si = 0
            while sh < win:
                dst, dstk = sAB[si % 2], ("sAB", si % 2)
                P.add('dve', lambda e, cur=cur, dst=dst, sh=sh: e.tensor_tensor(out=dst[:, 16:L], in0=cur[:, 16:L], in1=cur[:, 16 - sh:L - sh], op=ALU.add),
                      r=(curk,), w=(dstk,))
                cur, curk = dst, dstk
                sh *= 2
                si += 1
            P.add('dve', lambda e, cur=cur, g=g: e.tensor_tensor(out=cur[:, 144:160], in0=cur[:, 144:160], in1=corr[:, g * 16:(g + 1) * 16], op=ALU.mult),
                  r=(curk, "corr"), w=(curk,))
            P.add('dve', lambda e, cur=cur, hb=hb, kc=kc, win=win: e.scalar_tensor_tensor(out=pooledT[:, kc, :], in0=cur[:, 16:L], scalar=1.0 / win, in1=hb[:, 16:L],
                                                                                    op0=ALU.mult, op1=ALU.subtract),
                  r=(curk, ("hTf", hi)), w=(("pooledT", kc),))
        for t in range(1, NT):
            for g in range(4):
                bank = 4 + g // 2
                c0 = (g % 2) * 256
                def mm(e, t=t, g=g, bank=bank, c0=c0):
                    for k2 in range(2):
                        ins = e.matmul(ps[bank][:, c0:c0 + 256], pooledT[:, 2 * g + k2, t * 128:(t + 1) * 128], pw[:, g, k2, :], start=(k2 == 0), stop=(k2 == 1))
                    return ins
                P.add('pe', mm, r=(("pooledT", 2 * g), ("pooledT", 2 * g + 1), ("pw", g)), w=(("ps", bank),))
            epilogue(ns, [ps[4], ps[5]], [("ps", 4), ("ps", 5)], t, 1.0, psc=psc)

    def mixer0_phase():
        aX = Arena(nc, X_base, X_base + NT * D * 4)
        aR = Arena(nc, phase_base, ar.limit)
        NTOK = NT * 128
        kT = aX.alloc([128, 4, 4096], BF16)
        V = aX.alloc([128, 32, 8, 66], BF16)
        hT_all = aR.alloc([128, 8, NTOK], BF16)
        S0 = aR.top
        aR.top += 29216
        qT = aR.alloc([128, 4, NTOK], BF16)
        iqT = aR.alloc([128, 4, NTOK], BF16)
        aT = iqT
        cT = aR.alloc([128, 4, NTOK], BF16)
        iw_sb = aR.alloc([128, NT, 8], F32)
        ik2T = aR.alloc([128, 4096], BF16)
        lo_ = aR.alloc([128, 1], F32)
        mid_ = aR.alloc([128, 1], F32)
        cnt_ = aR.alloc([128, 1], F32)
        step_ = aR.alloc([128, 1], F32)
        mx_ = aR.alloc([128, 1], F32)
        mn_ = aR.alloc([128, 1], F32)
        w0_ = aR.alloc([128, 1], F32)
        wtab = aR.alloc([128, NBIS], F32)
        den = aR.alloc([128, 8], F32)
        rinv = aR.alloc([128, 8], F32)
        aH = Arena(nc, S0 - 8 * NTOK * 2, S0)
        aS = Arena(nc, S0, S0 + 29216)
        ns = alloc_ns(aS, nt=1)
        for t in range(NT):
            norm_transpose(ns, X[:, t, :], ("X", t), G_MIX[0], lambda kc, t=t: hT_all[:, kc, t * 128:(t + 1) * 128], ("hTa", t), 7)
            P.add('sp', lambda e, t=t: e.dma_start(out=xspill[t], in_=X[:, t, :]), r=(("X", t),), w=(("xsp", t),), dma=True, group="xsp0")
        P.barrier()
        aS = Arena(nc, S0, S0 + 29216)
        slots = [aS.alloc([128, 8, 128], BF16) for _ in range(3)]
        Wv_ = aS.alloc([128, 8, 512], BF16)
        Wiw = aS.alloc([128, 8, 8], BF16)
        pbuf = aS.alloc([128, 2 + NTOK], F32)
        ucopy = aS.alloc([128, 512], F32)
        ybuf = aS.alloc([128, 512], F32)
        winv = win_bf.rearrange("(kc p) f -> p kc f", p=128)
        P.add('pool', lambda e: e.memset(V.rearrange("p a b c -> p (a b c)"), 1.0), w=("V",))
        P.add('sp', lambda e: e.dma_start(out=kT[:, :, 0:2048], in_=kT_prev), r=("kTp",), w=("kTlo",), dma=True)
        P.add('sp', lambda e: e.dma_start(out=ik2T[:, 0:2048], in_=ik_prev), r=("ikp",), w=("iklo",), dma=True)
        for j in range(16):
            P.add('sp', lambda e, j=j: e.dma_start(out=V[:, j, :, 0:64], in_=v_prev[:, j, :].rearrange("p (h d) -> p h d", h=8)),
                  r=("vp", "V"), w=(("Vt", j),), dma=True, group="vlo")
        P.add('sp', lambda e: e.dma_start(out=Wv_, in_=winv[:, :, 1024:1536]), r=(("win",),), w=("mWv",), dma=True)
        P.add('sp', lambda e: e.dma_start(out=Wiw, in_=winv[:, :, 2112:2120]), r=(("win",),), w=("mWiw",), dma=True)
        sl = [0]

        def load_slot(col0, ncols=128, dst_off=0):
            k = sl[0] % 3
            P.add('sp', lambda e, k=k: e.dma_start(out=slots[k][:, :, dst_off:dst_off + ncols], in_=winv[:, :, col0:col0 + ncols]),
                  r=(("win",),), w=(("slot", k, dst_off),), dma=True)
            return k

        def next_slot():
            sl[0] += 1

        bk = [0]

        def proj_fm(k, rkeys, evac):
            for (t0, t1) in GROUPS17:
                T = (t1 - t0) * 128
                bank = bk[0] % 2
                bk[0] += 1
                def mm(e, t0=t0, T=T, bank=bank):
                    for kc in range(8):
                        ins = e.matmul(ps[bank][:, :T], slots[k][:, kc, :], hT_all[:, kc, t0 * 128:t0 * 128 + T], start=(kc == 0), stop=(kc == 7))
                    return ins
                P.add('pe', mm, r=rkeys + tuple(("hTa", t) for t in range(t0, t1)), w=(("ps", bank),))
                evac(bank, t0, t1, T)

        for ci in range(4):
            k = load_slot(ci * 128)
            next_slot()
            proj_fm(k, (("slot", k, 0),), lambda bank, t0, t1, T, ci=ci: P.add(
                'act', lambda e: e.activation(out=qT[:, ci, t0 * 128:t0 * 128 + T], in_=ps[bank][:, :T], func=AF.Copy, scale=0.125),
                r=(("ps", bank),), w=(("qT", ci),)))
        for ci in range(4):
            k = load_slot(1536 + ci * 128)
            next_slot()
            proj_fm(k, (("slot", k, 0),), lambda bank, t0, t1, T, ci=ci: P.add(
                'act', lambda e: e.activation(out=iqT[:, ci, t0 * 128:t0 * 128 + T], in_=ps[bank][:, :T], func=AF.Copy),
                r=(("ps", bank),), w=tuple(("iqT", t) for t in range(t0, t1))))
        for ci in range(4):
            k = load_slot(512 + ci * 128)
            next_slot()
            def ev(bank, t0, t1, T, ci=ci):
                if t0 == 0:
                    return
                P.add('act', lambda e: e.activation(out=kT[:, ci, 2048 + (t0 - 1) * 128:2048 + (t0 - 1) * 128 + T], in_=ps[bank][:, :T], func=AF.Copy),
                      r=(("ps", bank),), w=(("kThi", ci),))
            proj_fm(k, (("slot", k, 0),), ev)
        k = load_slot(2048, 64, 0)
        load_slot(2048, 64, 64)
        next_slot()
        def ev(bank, t0, t1, T):
            if t0 == 0:
                return
            P.add('act', lambda e: e.activation(out=ik2T[:, 2048 + (t0 - 1) * 128:2048 + (t0 - 1) * 128 + T], in_=ps[bank][:, :T], func=AF.Copy),
                  r=(("ps", bank),), w=("ikhi",))
        proj_fm(k, (("slot", k, 0), ("slot", k, 64)), ev)
        for t in range(NT):
            if t >= 1:
                def mm(e, t=t):
                    for kc in range(8):
                        ins = e.matmul(ps[2], hT_all[:, kc, t * 128:(t + 1) * 128], Wv_[:, kc, :], start=(kc == 0), stop=(kc == 7))
                    return ins
                P.add('pe', mm, r=("mWv", ("hTa", t)), w=(("ps", 2),))
                P.add('act', lambda e, t=t: e.activation(out=V[:, 15 + t, :, 0:64], in_=ps[2].rearrange("p (h d) -> p h d", h=8), func=AF.Copy),
                      r=(("ps", 2), "V"), w=(("Vt", 15 + t),))
            def mm(e, t=t):
                for kc in range(8):
                    ins = e.matmul(ps[3][:, 0:8], hT_all[:, kc, t * 128:(t + 1) * 128], Wiw[:, kc, :], start=(kc == 0), stop=(kc == 7))
                return ins
            P.add('pe', mm, r=("mWiw", ("hTa", t)), w=(("ps", 3),))
            P.add('dve', lambda e, t=t: e.tensor_scalar(out=iw_sb[:, t, :], in0=ps[3][:, 0:8], scalar1=float(512 ** -0.5), scalar2=None, op0=ALU.mult),
                  r=(("ps", 3),), w=(("iw", t),))
        P.add('dve', lambda e: e.memset(pbuf[:, 0:2], 0.0), w=("pbuf",))
        CWK = tuple(("cw", i) for i in range(12))
        for cc in range(4):
            ku = load_slot(2120 + cc * 128)
            next_slot()
            kgc = load_slot(3144 + cc * 128)
            next_slot()
            kgb = load_slot(2632 + cc * 128)
            next_slot()
            for (t0, t1) in GROUPS17:
                T = (t1 - t0) * 128
                a0 = t0 * 128
                for (kk, bank) in ((ku, 4), (kgc, 5), (kgb, 6)):
                    def mm(e, kk=kk, bank=bank, a0=a0, T=T):
                        for kc in range(8):
                            ins = e.matmul(ps[bank][:, :T], slots[kk][:, kc, :], hT_all[:, kc, a0:a0 + T], start=(kc == 0), stop=(kc == 7))
                        return ins
                    P.add('pe', mm, r=(("slot", kk, 0),) + tuple(("hTa", t) for t in range(t0, t1)), w=(("ps", bank),))
                P.add('act', lambda e, T=T: e.activation(out=ucopy[:, :T], in_=ps[4][:, :T], func=AF.Copy), r=(("ps", 4),), w=("ucopy",))
                P.add('dve', lambda e, T=T, a0=a0: e.tensor_tensor(out=pbuf[:, 2 + a0:2 + a0 + T], in0=ps[5][:, :T], in1=ucopy[:, :T], op=ALU.mult),
                      r=(("ps", 5), "ucopy"), w=("pbuf",))
                P.add('dve', lambda e, T=T, a0=a0, cc=cc: e.tensor_scalar(out=ybuf[:, :T], in0=pbuf[:, 2 + a0:2 + a0 + T], scalar1=cw[:, cc, 2:3], scalar2=None, op0=ALU.mult),
                      r=("pbuf",) + CWK, w=("ybuf",))
                P.add('dve', lambda e, T=T, a0=a0, cc=cc: e.scalar_tensor_tensor(out=ybuf[:, :T], in0=pbuf[:, 1 + a0:1 + a0 + T], scalar=cw[:, cc, 1:2], in1=ybuf[:, :T],
                                                                               op0=ALU.mult, op1=ALU.add), r=("pbuf", "ybuf"), w=("ybuf",))
                P.add('dve', lambda e, T=T, a0=a0, cc=cc: e.scalar_tensor_tensor(out=ybuf[:, :T], in0=pbuf[:, a0:a0 + T], scalar=cw[:, cc, 0:1], in1=ybuf[:, :T],
                                                                               op0=ALU.mult, op1=ALU.add), r=("pbuf", "ybuf"), w=("ybuf",))
                P.add('dve', lambda e, T=T, a0=a0, cc=cc: e.tensor_tensor(out=cT[:, cc, a0:a0 + T], in0=ps[6][:, :T], in1=ybuf[:, :T], op=ALU.mult),
                      r=(("ps", 6), "ybuf"), w=(("cT", cc),))
        P.barrier()
        if SUBSTOP == "m2":
            raise _Stop()
        aS = Arena(nc, S0, S0 + 29216)
        biasN = aS.alloc([128, 8, 256], F32)
        r_ = [aS.alloc([128, 512], F32) for _ in range(2)]
        ebuf = aS.alloc([128, 1024], F32)
        PT = [aS.alloc([128, 1024], BF16) for _ in range(2)]
        tmpb = aS.alloc([128, 1024], F32)
        a_tm = aS.alloc([128, 512], BF16)
        rb_sb = aS.alloc([32, 8], F32)
        oh_sb = aS.alloc([32, 383], F32)
        tv_sb = aS.alloc([8, 383], F32)
        score = aH.alloc([128, 4096], F32)
        mask = aH.alloc([128, 4096], BF16)
        maskT = aH.alloc([128, 32, 128], BF16)
        P.add('sp', lambda e: e.dma_start(out=rb_sb, in_=rel_bias), r=(), w=("rb_sb",), dma=True)
        P.add('sp', lambda e: e.dma_start(out=oh_sb, in_=c_onehot), r=(), w=("oh_sb",), dma=True)
        P.add('pe', lambda e: e.matmul(ps[0][0:8, 0:383], rb_sb, oh_sb, start=True, stop=True), r=("rb_sb", "oh_sb"), w=(("ps", 0),))
        P.add('act', lambda e: e.activation(out=tv_sb, in_=ps[0][0:8, 0:383], func=AF.Copy), r=(("ps", 0),), w=("tv_sb",))
        P.add('sp', lambda e: e.dma_start(out=tv_d, in_=tv_sb), r=("tv_sb",), w=("tv_d",), dma=True)
        for s_ in range(128):
            P.add('sp', lambda e, s_=s_: e.dma_start(out=biasN[s_:s_ + 1, :, :], in_=tv_d[:, 127 - s_:127 - s_ + 256].unsqueeze(0)),
                  r=("tv_d",), w=(("biasN_ld", s_),), dma=True, group="toe")
        for h in range(8):
            P.add('dve', lambda e, h=h: e.tensor_scalar(out=biasN[:, h, :], in0=biasN[:, h, :], scalar1=cfar[:, h:h + 1], scalar2=None, op0=ALU.subtract),
                  r=tuple(("biasN_ld", s_) for s_ in range(128)) + ("cfar",), w=(("biasN", h),))
        BIASK = tuple(("biasN", h) for h in range(8))
        if SUBSTOP == "m3":
            P.barrier()
            raise _Stop()
        ptc = [0]

        def EO(h):
            return (h % 2) * 512 + (h // 2) * 128

        def qtile(lt):
            vt = 15 + lt
            nk = vt + 1
            N = nk * 128
            q0 = lt * 128
            nch = (N + 511) // 512
            for ch in range(nch):
                k0 = ch * 512
                Kw = min(512, N - k0)
                for h in range(8):
                    hp = (h % 2) * 64
                    hb = h % 2
                    P.add('pe', lambda e, h=h, hp=hp, hb=hb, k0=k0, Kw=Kw: e.matmul(ps[hb][:, :Kw], iqT[hp:hp + 64, h // 2, q0:q0 + 128], ik2T[hp:hp + 64, k0:k0 + Kw],
                                                                                 start=True, stop=True),
                          r=(("iqT", lt), "iklo", "ikhi"), w=(("ps", hb),))
                    P.add('act', lambda e, hb=hb, Kw=Kw: e.activation(out=r_[hb][:, :Kw], in_=ps[hb][:, :Kw], func=AF.Relu), r=(("ps", hb),), w=(("r_", hb),))
                    if h == 0:
                        P.add('dve', lambda e, hb=hb, k0=k0, Kw=Kw: e.tensor_scalar(out=score[:, k0:k0 + Kw], in0=r_[hb][:, :Kw], scalar1=iw_sb[:, lt, 0:1], scalar2=None, op0=ALU.mult),
                              r=(("r_", hb), ("iw", lt)), w=("score",))
                    else:
                        P.add('dve', lambda e, hb=hb, k0=k0, Kw=Kw, h=h: e.scalar_tensor_tensor(out=score[:, k0:k0 + Kw], in0=r_[hb][:, :Kw], scalar=iw_sb[:, lt, h:h + 1],
                                                                                           in1=score[:, k0:k0 + Kw], op0=ALU.mult, op1=ALU.add),
                              r=(("r_", hb), ("iw", lt), "score"), w=("score",))
            if SUBSTOP == "m5_idx":
                P.barrier()
                raise _Stop()
            P.add('dve', lambda e, N=N: e.tensor_reduce(out=mx_, in_=score[:, :N], axis=AX.X, op=ALU.max), r=("score",), w=("mx",))
            P.add('dve', lambda e, N=N: e.tensor_reduce(out=mn_, in_=score[:, :N], axis=AX.X, op=ALU.min), r=("score",), w=("mn",))
            for ch in range(4):
                P.add('pe', lambda e, ch=ch: e.matmul(ps[2], onesb[0:1, :], kbrow[0:1, ch * 512:(ch + 1) * 512], start=True, stop=True),
                      r=("onesb", "kbrow"), w=(("ps", 2),))
                P.add('dve', lambda e, ch=ch: e.tensor_tensor(out=score[:, ch * 512:(ch + 1) * 512], in0=ps[2], in1=score[:, ch * 512:(ch + 1) * 512], op=ALU.add),
                      r=(("ps", 2), "score"), w=("score",))
            P.add('dve', lambda e, vt=vt: e.tensor_tensor(out=score[:, vt * 128:(vt + 1) * 128], in0=score[:, vt * 128:(vt + 1) * 128], in1=trif, op=ALU.add),
                  r=("score", "trif"), w=("score",))
            if SUBSTOP == "m5_msk":
                P.barrier()
                raise _Stop()
            P.add('dve', lambda e: e.tensor_tensor(out=w0_, in0=mx_, in1=mn_, op=ALU.subtract), r=("mx", "mn"), w=("w0",))
            P.add('dve', lambda e: e.tensor_scalar(out=w0_, in0=w0_, scalar1=2.0, scalar2=None, op0=ALU.add), r=("w0",), w=("w0",))
            P.add('dve', lambda e: e.tensor_scalar(out=lo_, in0=mn_, scalar1=-1.0, scalar2=None, op0=ALU.add), r=("mn",), w=("lo",))
            P.add('dve', lambda e: e.tensor_scalar(out=wtab, in0=cjtab, scalar1=w0_[:, 0:1], scalar2=None, op0=ALU.mult), r=("w0", "cjtab"), w=("wtab",))
            for j in range(NBIS):
                P.add('dve', lambda e, j=j: e.tensor_tensor(out=mid_, in0=lo_, in1=wtab[:, j:j + 1], op=ALU.add), r=("lo", "wtab"), w=("mid",))
                P.add('dve', lambda e, N=N: e.tensor_scalar(out=mask[:, :N], in0=score[:, :N], scalar1=mid_[:, 0:1], scalar2=None, op0=ALU.is_ge, op1=ALU.add, accum_out=cnt_),
                      r=("score", "mid"), w=("mask", "cnt"))
                P.add('dve', lambda e, j=j: e.tensor_scalar(out=step_, in0=cnt_, scalar1=256.0, scalar2=wtab[:, j:j + 1], op0=ALU.is_ge, op1=ALU.mult),
                      r=("cnt", "wtab"), w=("step",))
                P.add('dve', lambda e: e.tensor_tensor(out=lo_, in0=lo_, in1=step_, op=ALU.add), r=("lo", "step"), w=("lo",))
            P.add('dve', lambda e, N=N: e.tensor_scalar(out=mask[:, :N], in0=score[:, :N], scalar1=lo_[:, 0:1], scalar2=None, op0=ALU.is_ge), r=("score", "lo"), w=("mask",))
            if SUBSTOP == "m5_bis":
                P.barrier()
                raise _Stop()
            for jb in range(0, nk, 8):
                n8 = min(8, nk - jb)
                pb3 = psbf(3)
                def tp(e, jb=jb, n8=n8, pb3=pb3):
                    for i in range(n8):
                        ins = e.transpose(pb3[:, i * 128:(i + 1) * 128], mask[:, (jb + i) * 128:(jb + i + 1) * 128], identb)
                    return ins
                P.add('pe', tp, r=("mask", "identb"), w=(("ps", 3),))
                P.add('act', lambda e, jb=jb, n8=n8, pb3=pb3: e.activation(out=maskT[:, jb:jb + n8, :].rearrange("p a b -> p (a b)"), in_=pb3[:, :n8 * 128], func=AF.Copy),
                      r=(("ps", 3),), w=("maskT",))
            if SUBSTOP == "m5_mT":
                P.barrier()
                raise _Stop()
            for j in range(nk):
                near = (vt - j) <= 1
                off = (vt - j) * 128
                for h in range(8):
                    hp = (h % 2) * 64
                    bank = 4 + h % 2
                    P.add('pe', lambda e, h=h, hp=hp, bank=bank, j=j: e.matmul(ps[bank][:, (h // 2) * 128:(h // 2 + 1) * 128], kT[hp:hp + 64, h // 2, j * 128:(j + 1) * 128],
                                                                            qT[hp:hp + 64, h // 2, q0:q0 + 128], start=True, stop=True),
                          r=("kTlo", ("kThi", h // 2), ("qT", h // 2)), w=(("ps", bank),))
                if near:
                    for h in range(8):
                        bank = 4 + h % 2
                        P.add('dve', lambda e, h=h, bank=bank, off=off: e.tensor_tensor(out=tmpb[:, EO(h):EO(h) + 128], in0=ps[bank][:, (h // 2) * 128:(h // 2 + 1) * 128],
                                                                                   in1=biasN[:, h, off:off + 128], op=ALU.add),
                              r=(("ps", bank),) + BIASK, w=("tmpb",))
                    P.add('act', lambda e: e.activation(out=ebuf, in_=tmpb, func=AF.Exp), r=("tmpb",), w=("ebuf",))
                else:
                    for hh in range(2):
                        P.add('act', lambda e, hh=hh: e.activation(out=ebuf[:, hh * 512:(hh + 1) * 512], in_=ps[4 + hh], func=AF.Exp), r=(("ps", 4 + hh),), w=("ebuf",))
                if SUBSTOP == "att_exp" and j == 0:
                    P.barrier()
                    raise _Stop()
                pb = ptc[0] % 2
                ptc[0] += 1
                for h in range(8):
                    P.add('dve', lambda e, h=h, pb=pb, j=j: e.tensor_tensor(out=PT[pb][:, EO(h):EO(h) + 128], in0=ebuf[:, EO(h):EO(h) + 128], in1=maskT[:, j, :], op=ALU.mult),
                          r=("ebuf", "maskT"), w=(("PT", pb),))
                if SUBSTOP == "att_pt" and j == 0:
                    P.barrier()
                    raise _Stop()
                def pv(e, pb=pb, j=j, nk=nk):
                    for h in range(8):
                        ins = e.matmul(ps[6 + h // 4][:, (h % 4) * 66:(h % 4) * 66 + 66], PT[pb][:, EO(h):EO(h) + 128], V[:, j, h, :],
                                       start=(j == 0 and h % 4 == 0), stop=(j == nk - 1), skip_group_check=True)
                    return ins
                P.add('pe', pv, r=(("PT", pb), ("Vt", j), "V"), w=(("ps", 6), ("ps", 7)))
            if SUBSTOP == "m5_att":
                P.barrier()
                raise _Stop()
            for hh in range(2):
                P.add('dve', lambda e, hh=hh: e.tensor_scalar(out=den[:, hh * 4:(hh + 1) * 4], in0=ps[6 + hh][:, 0:264].rearrange("p (h c) -> p h c", c=66)[:, :, 64],
                                                             scalar1=1e-30, scalar2=None, op0=ALU.add), r=(("ps", 6 + hh),), w=(("den", hh),))
            P.add('dve', lambda e: e.reciprocal(out=rinv, in_=den), r=(("den", 0), ("den", 1)), w=("rinv",))
            for h in range(8):
                P.add('dve', lambda e, h=h: e.tensor_scalar(out=a_tm[:, h * 64:(h + 1) * 64], in0=ps[6 + h // 4][:, (h % 4) * 66:(h % 4) * 66 + 64], scalar1=rinv[:, h:h + 1],
                                                           scalar2=None, op0=ALU.mult), r=(("ps", 6 + h // 4), "rinv"), w=("a_tm",))
            pb3 = psbf(3)
            def tp(e, pb3=pb3):
                for ci in range(4):
                    ins = e.transpose(pb3[:, ci * 128:(ci + 1) * 128], a_tm[:, ci * 128:(ci + 1) * 128], identb)
                return ins
            P.add('pe', tp, r=("a_tm", "identb"), w=(("ps", 3),))
            for ci in range(4):
                P.add('act', lambda e, ci=ci, pb3=pb3: e.activation(out=aT[:, ci, q0:q0 + 128], in_=pb3[:, ci * 128:(ci + 1) * 128], func=AF.Copy),
                      r=(("ps", 3),), w=(("iqT", lt),))
            if SUBSTOP == "m5_one":
                P.barrier()
                raise _Stop()
        for lt_ in range(NT):
            qtile(lt_)
        P.barrier()
        aS = Arena(nc, S0, S0 + 29216)
        Wout = aS.alloc([128, 8, D], BF16)
        ns = alloc_ns(aS, nt=1)
        P.add('sp', lambda e: e.dma_start(out=Wout, in_=wout_bf.rearrange("(kc p) d -> p kc d", p=128)), r=(("wout",),), w=("Wout",), dma=True)
        load_gpost(mix_post[0])
        for t in range(NT):
            P.add('sp', lambda e, t=t: e.dma_start(out=X[:, t, :], in_=xspill[t]), r=(("xsp", t),), w=(("X", t),), dma=True, group="xrel0")
        for t in range(NT):
            for dh in range(2):
                def mm(e, t=t, dh=dh):
                    for kc in range(8):
                        src = aT if kc < 4 else cT
                        ins = e.matmul(ps[dh], src[:, kc % 4, t * 128:(t + 1) * 128], Wout[:, kc, dh * 512:(dh + 1) * 512], start=(kc == 0), stop=(kc == 7))
                    return ins
                P.add('pe', mm, r=(("iqT", t), "Wout") + tuple(("cT", c) for c in range(4)), w=(("ps", dh),))
            epilogue(ns, [ps[0], ps[1]], [("ps", 0), ("ps", 1)], t, 1.0)
        P.barrier()

    def load_own():
        for t in range(16):
            P.add('sp', lambda e, t=t: e.dma_start(out=X[:, 1 + t, :], in_=xo_v[:, t, :]), r=(), w=(("X", 1 + t),), dma=True, group="xown")

    def program():
        if STOP == "consts":
            load_own()
            return
        if STOP != "ffnown":
            PG, pre_load, after, a_holder = prev_ffn()
            pre_load(0)
            ffn_phase(0, 0, PG, after_group=after)
            P.barrier()
        load_own()
        ffn_phase(0, 0, GROUPS16)
        P.barrier()
        if STOP in ("ffn00", "ffnown"):
            return
        if STOP == "xa_only":
            xattn_phase(0, GROUPS17)
            P.barrier()
            return
        if STOP == "pool_only":
            pool_phase()
            P.barrier()
            return
        mixer0_phase()
        if STOP == "mix0":
            return
        xattn_phase(0, GROUPS17)
        P.barrier()
        if STOP == "xa0":
            return
        ffn_phase(0, 1, GROUPS17)
        P.barrier()
        if STOP == "ffn01":
            return
        ffn_phase(1, 0, GROUPS17)
        P.barrier()
        if STOP == "ffn10":
            return
        pool_phase()
        P.barrier()
        if STOP == "mix1":
            return
        xattn_phase(1, GROUPS16)
        P.barrier()
        if STOP == "xa1":
            return
        ffn_phase(1, 1, GROUPS16)
        P.barrier()

    try:
        program()
    except _Stop:
        P.barrier()

    for t in range(16):
        P.add('sp', lambda e, t=t: e.dma_start(out=out[t * 128:(t + 1) * 128, :], in_=X[:, 1 + t, :]),
              r=(("X", 1 + t),), w=(("out", t),), dma=True, group="outst")
    P.barrier()
    P.emit()
    return nc


def rest_of_program(env):
    return True


_CACHE = {}


def _consts(half):
    c = {}
    c["c_ident"] = np.eye(128, dtype=np.float32)
    q = np.arange(128)[:, None]
    s = np.arange(128)[None, :]
    c["c_tri"] = np.where(s > q, np.float32(-1e30), np.float32(0)).astype(np.float32)
    c["c_kb"] = np.full((1, 2048), 0.0 if half == 1 else -1e30, dtype=np.float32)
    e = np.arange(383) - 127
    b = t5_bucket_np(e)
    oh = np.zeros((32, 383), np.float32)
    oh[b, np.arange(383)] = 1.0
    c["c_onehot"] = oh
    c["c_halo"] = np.full((128, 1), float(half), dtype=np.float32)
    corr = np.ones((4, 16), np.float32)
    if half == 0:
        for g, win in enumerate((2, 4, 8, 16)):
            for t in range(16):
                corr[g, t] = win / min(t + 1, win)
    c["c_corr"] = np.broadcast_to(corr.reshape(1, 64), (128, 64)).copy()
    c["c_cj"] = np.broadcast_to((2.0 ** -(np.arange(NBIS) + 1.0)).astype(np.float32).reshape(1, NBIS), (128, NBIS)).copy()
    return c


WEIGHT_NAMES = ["ffn_w_gate", "ffn_w_up", "ffn_w_down", "ffn_norm_pre", "ffn_norm_post", "mix_norm_pre", "mix_norm_post",
                "even_w_in", "even_conv_w", "even_w_out", "rel_bias", "pool_w", "pool_scale", "xattn_norm_pre",
                "xattn_mem_norm", "xattn_norm_post", "xattn_wq", "xattn_wk", "xattn_wv", "xattn_wo"]


def kernel(**inputs):
    if "nc" not in _CACHE:
        _CACHE["nc"] = build()
    nc = _CACHE["nc"]
    x = np.ascontiguousarray(np.asarray(inputs["x"], dtype=np.float32))
    mem = np.ascontiguousarray(np.asarray(inputs["mem"], dtype=np.float32))
    shared = {k: np.ascontiguousarray(np.asarray(inputs[k], dtype=np.float32)) for k in WEIGHT_NAMES}
    zeros = np.zeros((2048, D), np.float32)
    in_maps = []
    for core in range(8):
        b, half = core // 2, core % 2
        m = dict(shared)
        m["x_own"] = np.ascontiguousarray(x[b, half * 2048:(half + 1) * 2048])
        m["x_prev"] = np.ascontiguousarray(x[b, 0:2048]) if half == 1 else zeros
        m["mem"] = np.ascontiguousarray(mem[b])
        m.update(_consts(half))
        in_maps.append(m)
    res = run_bass_kernel_spmd(nc, in_maps, core_ids=list(range(8)))
    outp = np.empty((4, 4096, D), np.float32)
    for core in range(8):
        b, half = core // 2, core % 2
        outp[b, half * 2048:(half + 1) * 2048] = res.results[core]["out"]
    return outp
```

```python
import numpy as np
import concourse.bass as bass
import concourse.mybir as mybir
from concourse.bass_utils import run_bass_kernel_spmd

F32 = mybir.dt.float32
BF16 = mybir.dt.bfloat16
ALU = mybir.AluOpType
AF = mybir.ActivationFunctionType
AX = mybir.AxisListType

NT = 17
D = 1024
DFF = 2816
NCH = 22
EPS = 1e-6
NBIS = 18
STOP = None


class _Op:
    pass


class _Stop(Exception):
    pass


SUBSTOP = None


class Prog:
    def __init__(self, nc):
        self.nc = nc
        self.ops = []
        self.lw = {}
        self.rd = {}
        self.eng_last = {}
        self.pend_dma = []

    def add(self, eng, fn, r=(), w=(), dma=False, group=None):
        i = len(self.ops)
        op = _Op()
        op.eng, op.fn, op.dma, op.group, op.idx = eng, fn, dma, group, i
        op.deps = set()
        op.wkey = w[0] if (dma and len(w)) else None
        for k in r:
            p = self.lw.get(k)
            if p is not None:
                op.deps.add(p)
        for k in w:
            p = self.lw.get(k)
            if p is not None:
                op.deps.add(p)
            rr = self.rd.get(k)
            if rr:
                op.deps.update(rr['c'].values())
                op.deps.update(rr['d'])
        for k in r:
            rr = self.rd.setdefault(k, {'c': {}, 'd': []})
            if dma:
                rr['d'].append(i)
            else:
                rr['c'][eng] = i
        for k in w:
            self.lw[k] = i
            self.rd[k] = {'c': {}, 'd': []}
        op.deps.discard(i)
        if group is not None:
            for d_ in op.deps:
                assert self.ops[d_].group != group, ("intra-group dependency", group, r, w)
        self.ops.append(op)
        if dma:
            self.pend_dma.append(i)
        else:
            self.eng_last[eng] = i
        return i

    def barrier(self):
        prev = set(self.eng_last.values()) | set(self.pend_dma)
        self.pend_dma = []
        for eng in ('pe', 'act', 'dve', 'pool', 'sp'):
            i = len(self.ops)
            op = _Op()
            op.eng, op.fn, op.dma, op.group, op.idx = eng, None, False, None, i
            op.deps = set(prev)
            op.wkey = None
            self.ops.append(op)
            self.eng_last[eng] = i

    def emit(self):
        nc = self.nc
        ops = self.ops
        for op in ops:
            op.deps = {d for d in op.deps
                       if not (op.eng == 'pe' and ops[d].eng == 'pe' and not op.dma and not ops[d].dma
                               and op.fn is not None and ops[d].fn is not None)}
        needed = set()
        for op in ops:
            needed.update(op.deps)
        cnt = {}
        for op in ops:
            if op.dma:
                key = ('g', op.group) if op.group else ('k', op.wkey)
                cnt[key] = cnt.get(key, 0) + 16
                op.sig = (key, cnt[key])
            elif op.idx in needed and op.fn is not None:
                key = ('e', op.eng)
                cnt[key] = cnt.get(key, 0) + 1
                op.sig = (key, cnt[key])
            else:
                op.sig = None
        def targets(d, acc):
            o = ops[d]
            if o.fn is None:
                for dd in o.deps:
                    targets(dd, acc)
            else:
                key, val = o.sig
                if key[0] == 'g':
                    val = cnt[key]
                if val > acc.get(key, 0):
                    acc[key] = val
        sems = {}
        for key in cnt:
            sems[key] = nc.alloc_semaphore(name="s%d" % len(sems))
        self.nsem = len(sems)
        done_sem = nc.alloc_semaphore(name="sdone")
        bar_cache = {}
        with nc.Block() as block:
            decos = {'sp': block.sync, 'act': block.scalar, 'dve': block.vector,
                     'pool': block.gpsimd, 'pe': block.tensor}
            for name, deco in decos.items():
                def body(e, name=name):
                    known = {}
                    for op in ops:
                        if op.eng != name:
                            continue
                        acc = {}
                        for d in op.deps:
                            if ops[d].fn is None:
                                if d not in bar_cache:
                                    a2 = {}
                                    targets(d, a2)
                                    bar_cache[d] = a2
                                for k2, v2 in bar_cache[d].items():
                                    if v2 > acc.get(k2, 0):
                                        acc[k2] = v2
                            else:
                                targets(d, acc)
                        for key, val in acc.items():
                            if known.get(key, 0) < val:
                                e.wait_ge(sems[key], val)
                                known[key] = val
                        if op.fn is None:
                            continue
                        ins = op.fn(e)
                        if op.sig is not None:
                            ins.then_inc(sems[op.sig[0]], 16 if op.dma else 1)
                    if name != 'sp':
                        e.sem_inc(done_sem, 1)
                    else:
                        e.wait_ge(done_sem, 4)
                        for sm in sems.values():
                            e.sem_clear(sm)
                        e.sem_clear(done_sem)
                deco(body)


class Arena:
    def __init__(self, nc, base, limit):
        self.nc, self.top, self.limit = nc, base, limit
        self.n = 0

    def alloc(self, shape, dt):
        sz = int(np.prod(shape[1:])) * (4 if dt == F32 else 2)
        sz = (sz + 31) // 32 * 32
        assert self.top + sz <= self.limit, ("SBUF overflow", self.top, sz, self.limit)
        self.n += 1
        t = self.nc.alloc_sbuf_tensor_at("sb%d" % self.n, list(shape), dt, offset=self.top).ap()
        self.top += sz
        return t


def t5_bucket_np(d):
    n = np.maximum(d, 0)
    nf = np.maximum(n, 1).astype(np.float32)
    large = 16 + (np.log(nf / np.float32(16)) / np.float32(np.log(128 / 16)) * np.float32(16)).astype(np.int32)
    large = np.minimum(large, 31)
    return np.where(n < 16, n, large)


def build():
    nc = bass.Bass("TRN2", target_bir_lowering=False)
    P = Prog(nc)

    def din(name, shape):
        return nc.dram_tensor(name, list(shape), F32, kind="ExternalInput").ap()

    x_own = din("x_own", [2048, D])
    x_prev = din("x_prev", [2048, D])
    mem = din("mem", [256, D])
    w_gate = din("ffn_w_gate", [2, 2, D, DFF])
    w_up = din("ffn_w_up", [2, 2, D, DFF])
    w_down = din("ffn_w_down", [2, 2, DFF, D])
    ffn_pre = din("ffn_norm_pre", [2, 2, D])
    ffn_post = din("ffn_norm_post", [2, 2, D])
    mix_pre = din("mix_norm_pre", [2, D])
    mix_post = din("mix_norm_post", [2, D])
    w_in = din("even_w_in", [1, D, 3656])
    conv_w = din("even_conv_w", [1, 3, 512])
    w_out = din("even_w_out", [1, D, D])
    rel_bias = din("rel_bias", [32, 8])
    pool_w = din("pool_w", [1, 4, 256, 256])
    pool_scale = din("pool_scale", [1, D])
    xa_pre = din("xattn_norm_pre", [2, D])
    xa_mem = din("xattn_mem_norm", [2, D])
    xa_post = din("xattn_norm_post", [2, D])
    xa_wq = din("xattn_wq", [2, D, 512])
    xa_wk = din("xattn_wk", [2, D, 512])
    xa_wv = din("xattn_wv", [2, D, 512])
    xa_wo = din("xattn_wo", [2, 512, D])
    c_ident = din("c_ident", [128, 128])
    c_tri = din("c_tri", [128, 128])
    c_kb = din("c_kb", [1, 2048])
    c_onehot = din("c_onehot", [32, 383])
    c_halo = din("c_halo", [128, 1])
    c_corr = din("c_corr", [128, 64])
    c_cj = din("c_cj", [128, NBIS])
    out = nc.dram_tensor("out", [2048, D], F32, kind="ExternalOutput").ap()

    def dscr(name, shape, dt=BF16):
        return nc.dram_tensor(name, list(shape), dt).ap()

    wg_bf = [[dscr("wg_bf%d%d" % (l, i), [D, DFF]) for i in range(2)] for l in range(2)]
    wu_bf = [[dscr("wu_bf%d%d" % (l, i), [D, DFF]) for i in range(2)] for l in range(2)]
    wd_bf = [[dscr("wd_bf%d%d" % (l, i), [DFF, D]) for i in range(2)] for l in range(2)]
    win_bf = dscr("win_bf", [D, 3656])
    wout_bf = dscr("wout_bf", [D, D])
    poolw_bf = dscr("poolw_bf", [4, 256, 256])
    xq_bf = [dscr("xq_bf%d" % l, [D, 512]) for l in range(2)]
    xk_bf = [dscr("xk_bf%d" % l, [D, 512]) for l in range(2)]
    xv_bf = [dscr("xv_bf%d" % l, [D, 512]) for l in range(2)]
    xo_bf = [dscr("xo_bf%d" % l, [512, D]) for l in range(2)]
    xspill = dscr("xspill", [NT, 128, D], F32)
    kT_prev = dscr("kT_prev", [128, 4, 2048])
    v_prev = dscr("v_prev", [128, 16, 512])
    ik_prev = dscr("ik_prev", [128, 2048])
    tv_d = dscr("tv_d", [8, 383], F32)

    import os
    SKIP = os.environ.get("KSKIP", "").split(",")

    def cast(dst, src, key):
        if "casts" in SKIP:
            return
        P.add('pool', lambda e: e.dma_start(out=dst, in_=src, max_dma_last_dim=4096), r=(), w=(key,), dma=True)

    cast_order = []

    WPC = [(0, 768), (768, 1536), (1536, 2304), (2304, 2816)]

    def wkey(kind, l, i, pc):
        return (kind, l, i, pc if (l, i) == (0, 0) else 0)

    def cast_ffn(l, i):
        if (l, i) == (0, 0):
            for pc, (c0, c1) in enumerate(WPC):
                cast(wg_bf[l][i][:, c0:c1], w_gate[l, i, :, c0:c1], ("wg", l, i, pc))
                cast(wu_bf[l][i][:, c0:c1], w_up[l, i, :, c0:c1], ("wu", l, i, pc))
            for q in range(2):
                cast(wd_bf[l][i][q * 1408:(q + 1) * 1408, :], w_down[l, i, q * 1408:(q + 1) * 1408, :], ("wd", l, i, q))
        else:
            cast(wg_bf[l][i], w_gate[l, i], ("wg", l, i, 0))
            cast(wu_bf[l][i], w_up[l, i], ("wu", l, i, 0))
            cast(wd_bf[l][i], w_down[l, i], ("wd", l, i, 0))

    def cast_xa(l):
        cast(xq_bf[l], xa_wq[l], ("xq", l))
        cast(xk_bf[l], xa_wk[l], ("xk", l))
        cast(xv_bf[l], xa_wv[l], ("xv", l))
        cast(xo_bf[l], xa_wo[l], ("xo", l))

    ar = Arena(nc, 16512, 229344)
    identb = ar.alloc([128, 128], BF16)
    identf = ar.alloc([128, 128], F32)
    onesb = ar.alloc([128, 128], BF16)
    trif = ar.alloc([128, 128], F32)
    cjtab = ar.alloc([128, NBIS], F32)
    kbrow = ar.alloc([1, 2048], BF16)
    gT = ar.alloc([128, 12, 8], F32)
    st = ar.alloc([128, 64], F32)
    negh = ar.alloc([128, 1], F32)
    cw = ar.alloc([128, 4, 3], F32)
    cfar = ar.alloc([128, 8], F32)
    halof = ar.alloc([128, 1], F32)
    corr = ar.alloc([128, 64], F32)
    gpost = ar.alloc([128, D], F32)
    X = ar.alloc([128, NT, D], F32)
    X_base = ar.top - NT * D * 4
    phase_base = ar.top

    ps = [nc.alloc_psum_tensor("psb%d" % i, [128, 512], F32).ap() for i in range(8)]

    def psbf(i):
        return ps[i].bitcast(BF16)

    def ld(dst, src, key, eng='sp', **kw):
        P.add(eng, lambda e: e.dma_start(out=dst, in_=src, **kw), r=(), w=(key,), dma=True, group=("consts" if eng == 'sp' else None))

    ld(identf, c_ident, "identf")
    ld(identb, c_ident, "identb", eng='pool')
    ld(trif, c_tri, "trif")
    ld(cjtab, c_cj, "cjtab")
    if "kb" not in SKIP:
        ld(kbrow, c_kb, "kbrow", eng='pool')
    ld(halof, c_halo, "halof")
    ld(corr, c_corr, "corr")
    if "cfar" not in SKIP:
      ld(cfar, rel_bias[31:32, :].partition_broadcast(128) if False else rel_bias[31, :].partition_broadcast(128), "cfar")
    norm_vecs = [ffn_pre[0, 0], ffn_pre[0, 1], ffn_pre[1, 0], ffn_pre[1, 1], mix_pre[0], mix_pre[1],
                 xa_pre[0], xa_pre[1], xa_mem[0], xa_mem[1]]
    G_FFN = {(0, 0): 0, (0, 1): 1, (1, 0): 2, (1, 1): 3}
    G_MIX = {0: 4, 1: 5}
    G_XA = {0: 6, 1: 7}
    G_MEM = {0: 8, 1: 9}
    for n, v in enumerate(norm_vecs):
        def f(e, n=n, v=v):
            with nc.allow_non_contiguous_dma(reason="tiny gain vector transpose"):
                return e.dma_start(out=gT[:, n, :], in_=v.rearrange("(k p) -> p k", p=128))
        P.add('sp', f, r=(), w=(("gT", n),), dma=True, group="consts")

    for j in range(3 if "cw" not in SKIP else 0):
        for c4 in range(4):
            def f(e, j=j, c4=c4):
                with nc.allow_non_contiguous_dma(reason="tiny conv weights"):
                    return e.dma_start(out=cw[:, c4, j:j + 1], in_=conv_w[0, j, c4 * 128:(c4 + 1) * 128].rearrange("(p o) -> p o", o=1))
            P.add('sp', f, r=(), w=(("cw", j * 4 + c4),), dma=True, group="consts")
    P.add('pool', lambda e: e.memset(negh, -0.5), w=("negh",))
    P.add('pool', lambda e: e.memset(onesb, 1.0), w=("onesb",))
    CONSTS = ("identf", "identb", "trib", "kbrow", "halof", "corr", "cfar", "cw", "negh", "onesb") + tuple(("gT", n) for n in range(10))

    cast_ffn(0, 0)
    cast(win_bf, w_in[0], ("win",))
    cast(wout_bf, w_out[0], ("wout",))
    cast_xa(0)
    cast_ffn(0, 1)
    cast_ffn(1, 0)
    cast(poolw_bf, pool_w[0], ("poolw",))
    cast_xa(1)
    cast_ffn(1, 1)

    stc = [0]

    def stslot(n=1):
        s = stc[0]
        stc[0] = (stc[0] + n) % 60
        if s + n > 60:
            s = 0
            stc[0] = n
        return s

    def rstd_from_ss(ss_ap, ss_key, out_ap, out_key, mul=1.0):
        m2 = mul * mul
        P.add('pool', lambda e: e.tensor_scalar(out=out_ap, in0=ss_ap, scalar1=1.0 / (D * m2), scalar2=EPS / m2, op0=ALU.mult, op1=ALU.add),
              r=(ss_key,), w=(out_key,))
        P.add('pool', lambda e: e.tensor_tensor(out=out_ap, in0=out_ap, in1=negh, op=ALU.pow), r=(out_key, "negh"), w=(out_key,))

    class NS:
        pass

    def alloc_ns(a, nt=2):
        ns = NS()
        ns.xn = [a.alloc([128, D], BF16) for _ in range(2)]
        ns.sqj = a.alloc([128, D], BF16)
        ns.t = [a.alloc([128, D], F32) for _ in range(nt)]
        ns.nt = nt
        ns.c = 0
        return ns

    def norm_transpose(ns, src_ap, src_key, gidx, dst_fn, dst_key, tpbank):
        s = stslot(2)
        b = ns.c % 2
        ns.c += 1
        xn = ns.xn[b]
        P.add('act', lambda e: e.activation(out=ns.sqj, in_=src_ap, func=AF.Square, accum_out=st[:, s:s + 1]),
              r=(src_key,), w=("sqj", ("st", s)))
        rstd_from_ss(st[:, s:s + 1], ("st", s), st[:, s + 1:s + 2], ("st", s + 1))
        P.add('dve', lambda e: e.tensor_scalar(out=xn, in0=src_ap, scalar1=st[:, s + 1:s + 2], scalar2=None, op0=ALU.mult),
              r=(src_key, ("st", s + 1)), w=(("xn", b),))
        pb = psbf(tpbank)

        def tp(e):
            for kc in range(8):
                ins = e.transpose(pb[:, kc * 128:(kc + 1) * 128], xn[:, kc * 128:(kc + 1) * 128], identb)
            return ins
        P.add('pe', tp, r=(("xn", b), "identb"), w=(("ps", tpbank),))

        def ev(e):
            for kc in range(8):
                ins = e.tensor_scalar(out=dst_fn(kc), in0=pb[:, kc * 128:(kc + 1) * 128], scalar1=gT[:, gidx, kc:kc + 1],
                                      scalar2=None, op0=ALU.mult)
            return ins
        P.add('dve', ev, r=(("ps", tpbank), ("gT", gidx)), w=(dst_key,))

    def load_gpost(vec):
        P.add('sp', lambda e: e.dma_start(out=gpost, in_=vec.partition_broadcast(128)), r=(), w=("gpost",), dma=True)

    def epilogue(ns, src_halves, src_keys, t, factor, psc=None):
        s = stslot(4)
        b = ns.c % ns.nt
        ns.c += 1
        tt = ns.t[b]
        if psc is not None:
            for hh in range(2):
                P.add('dve', lambda e, hh=hh: e.tensor_tensor(out=tt[:, hh * 512:(hh + 1) * 512], in0=src_halves[hh],
                                                              in1=psc[:, hh * 512:(hh + 1) * 512], op=ALU.mult),
                      r=(src_keys[hh], "psc"), w=(("t", b),))
            srcs = [tt[:, 0:512], tt[:, 512:1024]]
            skeys = [("t", b), ("t", b)]
        else:
            srcs, skeys = src_halves, src_keys
        for hh in range(2):
            P.add('act', lambda e, hh=hh: e.activation(out=ns.sqj[:, hh * 512:(hh + 1) * 512], in_=srcs[hh], func=AF.Square,
                                                        accum_out=st[:, s + hh:s + hh + 1]),
                  r=(skeys[hh],), w=("sqj", ("st", s + hh)))
        P.add('dve', lambda e: e.tensor_tensor(out=st[:, s + 2:s + 3], in0=st[:, s:s + 1], in1=st[:, s + 1:s + 2], op=ALU.add),
              r=(("st", s), ("st", s + 1)), w=(("st", s + 2),))
        rstd_from_ss(st[:, s + 2:s + 3], ("st", s + 2), st[:, s + 3:s + 4], ("st", s + 3), mul=factor)
        for hh in range(2):
            P.add('dve', lambda e, hh=hh: e.tensor_tensor(out=tt[:, hh * 512:(hh + 1) * 512], in0=srcs[hh],
                                                          in1=gpost[:, hh * 512:(hh + 1) * 512], op=ALU.mult),
                  r=(skeys[hh], "gpost"), w=(("t", b),))
        P.add('dve', lambda e: e.scalar_tensor_tensor(out=X[:, t, :], in0=tt, scalar=st[:, s + 3:s + 4], in1=X[:, t, :],
                                                      op0=ALU.mult, op1=ALU.add),
              r=(("t", b), ("st", s + 3), ("X", t)), w=(("X", t),))

    def ffn_phase(l, i, groups, first_src=None, after_group=None):
        a = Arena(nc, phase_base, ar.limit)
        Wd = a.alloc([128, NCH, D], BF16)
        actT = a.alloc([128, NCH, 512], BF16)
        hT = a.alloc([128, 8, 512], BF16)
        wgu = [a.alloc([128, 2, 8, 256], BF16) for _ in range(2)]
        sg = [a.alloc([128, 512], F32) for _ in range(2)]
        ns = alloc_ns(a)
        wdv = wd_bf[l][i].rearrange("(c p) d -> p c d", p=128)
        for q in range(2):
            P.add('sp', lambda e, q=q: e.dma_start(out=Wd[:, q * 11:(q + 1) * 11, :], in_=wdv[:, q * 11:(q + 1) * 11, :]),
                  r=(wkey("wd", l, i, q),), w=(("Wd", q),), dma=True)
        load_gpost(ffn_post[l, i])
        wgv = wg_bf[l][i].rearrange("(kc p) f -> p kc f", p=128)
        wuv = wu_bf[l][i].rearrange("(kc p) f -> p kc f", p=128)
        gidx = G_FFN[(l, i)]
        sc_count = [0]
        for gi, (t0, t1) in enumerate(groups):
            n = t1 - t0
            T = n * 128
            for j in range(n):
                norm_transpose(ns, X[:, t0 + j, :], ("X", t0 + j), gidx,
                               lambda kc, j=j: hT[:, kc, j * 128:(j + 1) * 128], "hT", 7)
            if SUBSTOP == "nt":
                raise _Stop()
            for sc in range(11):
                slot = sc_count[0] % 2
                sc_count[0] += 1
                P.add('sp', lambda e, sc=sc, slot=slot: e.dma_start(out=wgu[slot][:, 0], in_=wgv[:, :, sc * 256:(sc + 1) * 256]),
                      r=(wkey("wg", l, i, sc // 3),), w=(("wgu", slot, 0),), dma=True)
                P.add('sp', lambda e, sc=sc, slot=slot: e.dma_start(out=wgu[slot][:, 1], in_=wuv[:, :, sc * 256:(sc + 1) * 256]),
                      r=(wkey("wu", l, i, sc // 3),), w=(("wgu", slot, 1),), dma=True)
                for hh in range(2):
                    c = sc * 2 + hh
                    bk = c % 2
                    for which in range(2):
                        bank = which * 2 + bk

                        def mm(e, which=which, bank=bank, slot=slot, hh=hh):
                            for kc in range(8):
                                ins = e.matmul(ps[bank][:, :T], wgu[slot][:, which, kc, hh * 128:(hh + 1) * 128], hT[:, kc, :T],
                                               start=(kc == 0), stop=(kc == 7))
                            return ins
                        P.add('pe', mm, r=(("wgu", slot, which), "hT"), w=(("ps", bank),))
                    P.add('act', lambda e, bk=bk: e.activation(out=sg[bk][:, :T], in_=ps[bk][:, :T], func=AF.Silu),
                          r=(("ps", bk),), w=(("sg", bk),))
                    P.add('dve', lambda e, bk=bk, c=c: e.tensor_tensor(out=actT[:, c, :T], in0=ps[2 + bk][:, :T], in1=sg[bk][:, :T], op=ALU.mult),
                          r=(("ps", 2 + bk), ("sg", bk)), w=(("actT", c),))
            if SUBSTOP == "pa":
                raise _Stop()
            for j in range(n):
                banks = (4, 5) if j % 2 == 0 else (6, 7)
                for dh in range(2):
                    def mm(e, j=j, dh=dh, bank=banks[dh]):
                        for c in range(NCH):
                            ins = e.matmul(ps[bank], actT[:, c, j * 128:(j + 1) * 128], Wd[:, c, dh * 512:(dh + 1) * 512],
                                           start=(c == 0), stop=(c == NCH - 1))
                        return ins
                    P.add('pe', mm, r=tuple(("actT", c) for c in range(NCH)) + (("Wd", 0), ("Wd", 1)), w=(("ps", banks[dh]),))
                epilogue(ns, [ps[banks[0]], ps[banks[1]]], [("ps", banks[0]), ("ps", banks[1])], t0 + j, 0.5)
            if SUBSTOP == "pb":
                raise _Stop()
            if after_group is not None:
                after_group(gi, a, ns, hT, actT)

    xo_v = x_own.rearrange("(t p) d -> p t d", p=128)
    xp_v = x_prev.rearrange("(t p) d -> p t d", p=128)

    def prev_pass():
        pass

    GROUPS17 = [(0, 1), (1, 5), (5, 9), (9, 13), (13, 17)]
    GROUPS16 = [(1, 5), (5, 9), (9, 13), (13, 17)]

    def prev_ffn():
        PG = [(13, 17)] * 4
        winv = win_bf.rearrange("(kc p) f -> p kc f", p=128)

        def pre_load(gi):
            for j in range(4):
                P.add('sp', lambda e, j=j, gi=gi: e.dma_start(out=X[:, 13 + j, :], in_=xp_v[:, gi * 4 + j, :]),
                      r=(), w=(("X", 13 + j),), dma=True, group=("xprev", gi))

        a_holder = {}

        def after(gi, a, nsl, hTp, actT):
            if 'wk' not in a_holder:
                a_holder['wk'] = a.alloc([128, 8, 512], BF16)
                a_holder['wv'] = a.alloc([128, 8, 512], BF16)
                a_holder['wik'] = a.alloc([128, 8, 128], BF16)
                P.add('sp', lambda e: e.dma_start(out=a_holder['wk'], in_=winv[:, :, 512:1024]), r=(("win",),), w=("pwk",), dma=True)
                P.add('sp', lambda e: e.dma_start(out=a_holder['wv'], in_=winv[:, :, 1024:1536]), r=(("win",),), w=("pwv",), dma=True)
                P.add('sp', lambda e: e.dma_start(out=a_holder['wik'][:, :, 0:64], in_=winv[:, :, 2048:2112]), r=(("win",),), w=("pwik0",), dma=True)
                P.add('sp', lambda e: e.dma_start(out=a_holder['wik'][:, :, 64:128], in_=winv[:, :, 2048:2112]), r=(("win",),), w=("pwik1",), dma=True)
            wk, wv, wik = a_holder['wk'], a_holder['wv'], a_holder['wik']
            ko, vo, iko = actT[:, 0:4, :], actT[:, 4:8, :], actT[:, 8, :]
            KO = tuple(("actT", c) for c in range(0, 4))
            VO = tuple(("actT", c) for c in range(4, 8))
            IKO = (("actT", 8),)
            for j in range(4):
                norm_transpose(nsl, X[:, 13 + j, :], ("X", 13 + j), G_MIX[0],
                               lambda kc, j=j: hTp[:, kc, j * 128:(j + 1) * 128], "hT", 7)
            for ci in range(4):
                bank = ci % 2
                def mm(e, ci=ci, bank=bank):
                    for kc in range(8):
                        ins = e.matmul(ps[bank], wk[:, kc, ci * 128:(ci + 1) * 128], hTp[:, kc, :], start=(kc == 0), stop=(kc == 7))
                    return ins
                P.add('pe', mm, r=("pwk", "hT"), w=(("ps", bank),))
                P.add('act', lambda e, ci=ci, bank=bank: e.activation(out=ko[:, ci, :], in_=ps[bank], func=AF.Copy), r=(("ps", bank),), w=(("actT", ci),))
            def mm(e):
                for kc in range(8):
                    ins = e.matmul(ps[2], wik[:, kc, :], hTp[:, kc, :], start=(kc == 0), stop=(kc == 7))
                return ins
            P.add('pe', mm, r=("pwik0", "pwik1", "hT"), w=(("ps", 2),))
            P.add('act', lambda e: e.activation(out=iko, in_=ps[2], func=AF.Copy), r=(("ps", 2),), w=IKO)
            for j in range(4):
                bank = 4 + j % 2
                def mm(e, j=j, bank=bank):
                    for kc in range(8):
                        ins = e.matmul(ps[bank], hTp[:, kc, j * 128:(j + 1) * 128], wv[:, kc, :], start=(kc == 0), stop=(kc == 7))
                    return ins
                P.add('pe', mm, r=("pwv", "hT"), w=(("ps", bank),))
                P.add('act', lambda e, j=j, bank=bank: e.activation(out=vo[:, j, :], in_=ps[bank], func=AF.Copy), r=(("ps", bank),), w=(("actT", 4 + j),))
            P.add('sp', lambda e, gi=gi: e.dma_start(out=kT_prev[:, :, gi * 512:(gi + 1) * 512], in_=ko), r=KO, w=("kTp",), dma=True)
            P.add('sp', lambda e, gi=gi: e.dma_start(out=v_prev[:, gi * 4:(gi + 1) * 4, :], in_=vo), r=VO, w=("vp",), dma=True)
            P.add('sp', lambda e, gi=gi: e.dma_start(out=ik_prev[:, gi * 512:(gi + 1) * 512], in_=iko), r=IKO, w=("ikp",), dma=True)
            if gi < 3:
                pre_load(gi + 1)
            else:
                P.add('pool', lambda e: e.tensor_copy(out=X[:, 0, :], in_=X[:, 16, :]), r=(("X", 16),), w=(("X", 0),))
        return PG, pre_load, after, a_holder

    stage = {}

    def stop_here(name):
        return STOP == name


    def xattn_phase(l, groups):
        a = Arena(nc, phase_base, ar.limit)
        memx = a.alloc([128, 2, D], F32)
        memT = a.alloc([128, 8, 256], BF16)
        kxT = a.alloc([128, 4, 256], BF16)
        vx = a.alloc([128, 2, 512], BF16)
        Wq = a.alloc([128, 8, 512], BF16)
        Wk = a.alloc([128, 8, 512], BF16)
        Wv = a.alloc([128, 8, 512], BF16)
        Wo = a.alloc([128, 4, D], BF16)
        hT = a.alloc([128, 8, 512], BF16)
        qxT = a.alloc([128, 4, 512], BF16)
        PTx = [a.alloc([128, 2, 512], BF16) for _ in range(2)]
        rs = a.alloc([128, 512], F32)
        oTn = a.alloc([128, 4, 512], BF16)
        ns = alloc_ns(a)
        P.add('sp', lambda e: e.dma_start(out=Wq, in_=xq_bf[l].rearrange("(kc p) f -> p kc f", p=128)), r=(("xq", l),), w=("xWq",), dma=True)
        P.add('sp', lambda e: e.dma_start(out=Wk, in_=xk_bf[l].rearrange("(kc p) f -> p kc f", p=128)), r=(("xk", l),), w=("xWk",), dma=True)
        P.add('sp', lambda e: e.dma_start(out=Wv, in_=xv_bf[l].rearrange("(kc p) f -> p kc f", p=128)), r=(("xv", l),), w=("xWv",), dma=True)
        P.add('sp', lambda e: e.dma_start(out=Wo, in_=xo_bf[l].rearrange("(h p) d -> p h d", p=128)), r=(("xo", l),), w=("xWo",), dma=True)
        P.add('sp', lambda e: e.dma_start(out=memx, in_=mem.rearrange("(t p) d -> p t d", p=128)), r=(), w=("memx",), dma=True)
        load_gpost(xa_post[l])
        for mt in range(2):
            norm_transpose(ns, memx[:, mt, :], "memx", G_MEM[l], lambda kc, mt=mt: memT[:, kc, mt * 128:(mt + 1) * 128], "memT", 7)
        for h in range(4):
            bank = h % 2
            def mm(e, h=h, bank=bank):
                for kc in range(8):
                    ins = e.matmul(ps[bank][:, :256], Wk[:, kc, h * 128:(h + 1) * 128], memT[:, kc, :], start=(kc == 0), stop=(kc == 7))
                return ins
            P.add('pe', mm, r=("xWk", "memT"), w=(("ps", bank),))
            P.add('act', lambda e, h=h, bank=bank: e.activation(out=kxT[:, h, :], in_=ps[bank][:, :256], func=AF.Copy), r=(("ps", bank),), w=("kxT",))
        for mt in range(2):
            bank = 2 + mt
            def mm(e, mt=mt, bank=bank):
                for kc in range(8):
                    ins = e.matmul(ps[bank], memT[:, kc, mt * 128:(mt + 1) * 128], Wv[:, kc, :], start=(kc == 0), stop=(kc == 7))
                return ins
            P.add('pe', mm, r=("xWv", "memT"), w=(("ps", bank),))
            P.add('act', lambda e, mt=mt, bank=bank: e.activation(out=vx[:, mt, :], in_=ps[bank], func=AF.Copy), r=(("ps", bank),), w=("vx",))
        cnt = [0]
        for (t0, t1) in groups:
            n = t1 - t0
            T = n * 128
            for j in range(n):
                norm_transpose(ns, X[:, t0 + j, :], ("X", t0 + j), G_XA[l], lambda kc, j=j: hT[:, kc, j * 128:(j + 1) * 128], "hT", 7)
            for h in range(4):
                pb = cnt[0] % 2
                cnt[0] += 1
                def mm(e, h=h):
                    for kc in range(8):
                        ins = e.matmul(ps[0][:, :T], Wq[:, kc, h * 128:(h + 1) * 128], hT[:, kc, :T], start=(kc == 0), stop=(kc == 7))
                    return ins
                P.add('pe', mm, r=("xWq", "hT"), w=(("ps", 0),))
                P.add('act', lambda e, h=h: e.activation(out=qxT[:, h, :T], in_=ps[0][:, :T], func=AF.Copy, scale=float(128 ** -0.5)),
                      r=(("ps", 0),), w=(("qxT", h),))
                for mt in range(2):
                    P.add('pe', lambda e, h=h, mt=mt: e.matmul(ps[1 + mt][:, :T], kxT[:, h, mt * 128:(mt + 1) * 128], qxT[:, h, :T], start=True, stop=True),
                          r=("kxT", ("qxT", h)), w=(("ps", 1 + mt),))
                    P.add('act', lambda e, mt=mt, pb=pb: e.activation(out=PTx[pb][:, mt, :T], in_=ps[1 + mt][:, :T], func=AF.Exp),
                          r=(("ps", 1 + mt),), w=(("PTx", pb, mt),))
                def mm(e, pb=pb):
                    for mt in range(2):
                        ins = e.matmul(ps[3][:, :T], onesb, PTx[pb][:, mt, :T], start=(mt == 0), stop=(mt == 1))
                    return ins
                P.add('pe', mm, r=("onesb", ("PTx", pb, 0), ("PTx", pb, 1)), w=(("ps", 3),))
                def mm(e, pb=pb, h=h):
                    for mt in range(2):
                        ins = e.matmul(ps[4][:, :T], vx[:, mt, h * 128:(h + 1) * 128], PTx[pb][:, mt, :T], start=(mt == 0), stop=(mt == 1))
                    return ins
                P.add('pe', mm, r=("vx", ("PTx", pb, 0), ("PTx", pb, 1)), w=(("ps", 4),))
                P.add('dve', lambda e: e.reciprocal(out=rs[:, :T], in_=ps[3][:, :T]), r=(("ps", 3),), w=("rs",))
                P.add('dve', lambda e, h=h: e.tensor_tensor(out=oTn[:, h, :T], in0=ps[4][:, :T], in1=rs[:, :T], op=ALU.mult),
                      r=(("ps", 4), "rs"), w=(("oTn", h),))
            for j in range(n):
                for dh in range(2):
                    def mm(e, j=j, dh=dh):
                        for h in range(4):
                            ins = e.matmul(ps[5 + dh], oTn[:, h, j * 128:(j + 1) * 128], Wo[:, h, dh * 512:(dh + 1) * 512], start=(h == 0), stop=(h == 3))
                        return ins
                    P.add('pe', mm, r=tuple(("oTn", h) for h in range(4)) + ("xWo",), w=(("ps", 5 + dh),))
                epilogue(ns, [ps[5], ps[6]], [("ps", 5), ("ps", 6)], t0 + j, 1.0)

    def pool_phase():
        a = Arena(nc, phase_base, ar.limit)
        L = 16 + NT * 128
        hTf = [a.alloc([128, L], F32) for _ in range(2)]
        sAB = [a.alloc([128, L], F32) for _ in range(2)]
        pooledT = a.alloc([128, 8, NT * 128], BF16)
        pw = a.alloc([128, 4, 2, 256], BF16)
        psc = a.alloc([128, D], F32)
        rstd17 = a.alloc([128, NT], F32)
        xs = [a.alloc([128, 128], F32) for _ in range(2)]
        ns = alloc_ns(a)
        for g in range(4):
            P.add('sp', lambda e, g=g: e.dma_start(out=pw[:, g], in_=poolw_bf[g].rearrange("(k p) d -> p k d", p=128)),
                  r=(("poolw",),), w=(("pw", g),), dma=True)
        P.add('sp', lambda e: e.dma_start(out=psc, in_=pool_scale[0].partition_broadcast(128)), r=(), w=("psc",), dma=True)
        load_gpost(mix_post[1])
        for t in range(NT):
            s_ = stslot(1)
            P.add('act', lambda e, t=t, s_=s_: e.activation(out=ns.sqj, in_=X[:, t, :], func=AF.Square, accum_out=st[:, s_:s_ + 1]),
                  r=(("X", t),), w=("sqj", ("st", s_)))
            rstd_from_ss(st[:, s_:s_ + 1], ("st", s_), rstd17[:, t:t + 1], ("rstd17", t))
        for i2 in range(2):
            P.add('dve', lambda e, i2=i2: e.memset(hTf[i2][:, 0:16], 0.0), w=(("hTf", i2),))
            P.add('dve', lambda e, i2=i2: e.memset(sAB[i2][:, 0:16], 0.0), w=(("sAB", i2),))
        xc = [0]
        for kc in range(8):
            g = kc // 2
            win = (2, 4, 8, 16)[g]
            hi = kc % 2
            hb = hTf[hi]
            for t in range(NT):
                b = xc[0] % 2
                xc[0] += 1
                bank = (t // 4) % 2
                P.add('dve', lambda e, t=t, b=b, kc=kc: e.tensor_scalar(out=xs[b], in0=X[:, t, kc * 128:(kc + 1) * 128], scalar1=rstd17[:, t:t + 1],
                                                                       scalar2=None, op0=ALU.mult),
                      r=(("X", t), ("rstd17", t)), w=(("xs", b),))
                P.add('pe', lambda e, t=t, b=b, bank=bank: e.transpose(ps[bank][:, (t % 4) * 128:(t % 4 + 1) * 128], xs[b], identf),
                      r=(("xs", b), "identf"), w=(("ps", bank),))
                if t % 4 == 3 or t == NT - 1:
                    tb = (t // 4) * 4
                    wd_ = (t - tb + 1) * 128
                    P.add('dve', lambda e, tb=tb, wd_=wd_, bank=bank, hb=hb, kc=kc: e.tensor_scalar(
                        out=hb[:, 16 + tb * 128:16 + tb * 128 + wd_], in0=ps[bank][:, :wd_], scalar1=gT[:, G_MIX[1], kc:kc + 1], scalar2=None, op0=ALU.mult),
                        r=(("ps", bank), ("gT", G_MIX[1])), w=(("hTf", hi),))
            P.add('dve', lambda e, hb=hb: e.tensor_scalar(out=hb[:, 16:144], in0=hb[:, 16:144], scalar1=halof[:, 0:1], scalar2=None, op0=ALU.mult),
                  r=(("hTf", hi), "halof"), w=(("hTf", hi),))
            cur, curk = hb, ("hTf", hi)
            sh = 1
            si = 0
            while sh < win:
                dst, dstk = sAB[si % 2], ("sAB", si % 2)
                P.add('dve', lambda e, cur=cur, dst=dst, sh=sh: e.tensor_tensor(out=dst[:, 16:L], in0=cur[:, 16:L], in1=cur[:, 16 - sh:L - sh], op=ALU.add),
                      r=(curk,), w=(dstk,))
                cur, curk = dst, dstk
                sh *= 2
                si += 1
            P.add('dve', lambda e, cur=cur, g=g: e.tensor_tensor(out=cur[:, 144:160], in0=cur[:, 144:160], in1=corr[:, g * 16:(g + 1) * 16], op=ALU.mult),
                  r=(curk, "corr"), w=(curk,))
            P.add('dve', lambda e, cur=cur, hb=hb, kc=kc, win=win: e.scalar_tensor_tensor(out=pooledT[:, kc, :], in0=cur[:, 16:L], scalar=1.0 / win, in1=hb[:, 16:L],
                                                                                    op0=ALU.mult, op1=ALU.subtract),
                  r=(curk, ("hTf", hi)), w=(("pooledT", kc),))
        for t in range(1, NT):
            for g in range(4):
                bank = 4 + g // 2
                c0 = (g % 2) * 256
                def mm(e, t=t, g=g, bank=bank, c0=c0):
                    for k2 in range(2):
                        ins = e.matmul(ps[bank][:, c0:c0 + 256], pooledT[:, 2 * g + k2, t * 128:(t + 1) * 128], pw[:, g, k2, :], start=(k2 == 0), stop=(k2 == 1))
                    return ins
                P.add('pe', mm, r=(("pooledT", 2 * g), ("pooledT", 2 * g + 1), ("pw", g)), w=(("ps", bank),))
            epilogue(ns, [ps[4], ps[5]], [("ps", 4), ("ps", 5)], t, 1.0, psc=psc)

    def mixer0_phase():
        aX = Arena(nc, X_base, X_base + NT * D * 4)
        aR = Arena(nc, phase_base, ar.limit)
        NTOK = NT * 128
        kT = aX.alloc([128, 4, 4096], BF16)
        V = aX.alloc([128, 32, 8, 66], BF16)
        hT_all = aR.alloc([128, 8, NTOK], BF16)
        S0 = aR.top
        aR.top += 29728
        qT = aR.alloc([128, 4, NTOK], BF16)
        iqT = aR.alloc([128, 4, NTOK], BF16)
        aT = iqT
        cT = aR.alloc([128, 4, NTOK], BF16)
        iw_sb = aR.alloc([128, NT, 8], F32)
        ik2T = aR.alloc([128, 4096], BF16)
        lo_ = aR.alloc([128, 1], F32)
        mid_ = aR.alloc([128, 1], F32)
        cnt_ = aR.alloc([128, 1], F32)
        step_ = aR.alloc([128, 1], F32)
        mx_ = aR.alloc([128, 1], F32)
        mn_ = aR.alloc([128, 1], F32)
        w0_ = aR.alloc([128, 1], F32)
        wtab = aR.alloc([128, NBIS], F32)
        den = aR.alloc([128, 8], F32)
        rinv = aR.alloc([128, 8], F32)
        aH = Arena(nc, S0 - 8 * NTOK * 2, S0)
        aS = Arena(nc, S0, S0 + 29728)
        ns = alloc_ns(aS, nt=1)
        for t in range(NT):
            norm_transpose(ns, X[:, t, :], ("X", t), G_MIX[0], lambda kc, t=t: hT_all[:, kc, t * 128:(t + 1) * 128], ("hTa", t), 7)
            P.add('sp', lambda e, t=t: e.dma_start(out=xspill[t], in_=X[:, t, :]), r=(("X", t),), w=(("xsp", t),), dma=True, group="xsp0")
        P.barrier()
        aS = Arena(nc, S0, S0 + 29728)
        slots = [aS.alloc([128, 8, 128], BF16) for _ in range(3)]
        Wv_ = aS.alloc([128, 8, 512], BF16)
        Wiw = aS.alloc([128, 8, 8], BF16)
        pbuf = aS.alloc([128, 2 + NTOK], F32)
        ucopy = aS.alloc([128, 512], F32)
        ybuf = aS.alloc([128, 512], F32)
        winv = win_bf.rearrange("(kc p) f -> p kc f", p=128)
        P.add('pool', lambda e: e.memset(V.rearrange("p a b c -> p (a b c)"), 1.0), w=("V",))
        P.add('sp', lambda e: e.dma_start(out=kT[:, :, 0:2048], in_=kT_prev), r=("kTp",), w=("kTlo",), dma=True)
        P.add('sp', lambda e: e.dma_start(out=ik2T[:, 0:2048], in_=ik_prev), r=("ikp",), w=("iklo",), dma=True)
        for j in range(16):
            P.add('sp', lambda e, j=j: e.dma_start(out=V[:, j, :, 0:64], in_=v_prev[:, j, :].rearrange("p (h d) -> p h d", h=8)),
                  r=("vp", "V"), w=(("Vt", j),), dma=True, group="vlo")
        P.add('sp', lambda e: e.dma_start(out=Wv_, in_=winv[:, :, 1024:1536]), r=(("win",),), w=("mWv",), dma=True)
        P.add('sp', lambda e: e.dma_start(out=Wiw, in_=winv[:, :, 2112:2120]), r=(("win",),), w=("mWiw",), dma=True)
        sl = [0]

        def load_slot(col0, ncols=128, dst_off=0):
            k = sl[0] % 3
            P.add('sp', lambda e, k=k: e.dma_start(out=slots[k][:, :, dst_off:dst_off + ncols], in_=winv[:, :, col0:col0 + ncols]),
                  r=(("win",),), w=(("slot", k, dst_off),), dma=True)
            return k

        def next_slot():
            sl[0] += 1

        bk = [0]

        def proj_fm(k, rkeys, evac):
            for (t0, t1) in GROUPS17:
                T = (t1 - t0) * 128
                bank = bk[0] % 2
                bk[0] += 1
                def mm(e, t0=t0, T=T, bank=bank):
                    for kc in range(8):
                        ins = e.matmul(ps[bank][:, :T], slots[k][:, kc, :], hT_all[:, kc, t0 * 128:t0 * 128 + T], start=(kc == 0), stop=(kc == 7))
                    return ins
                P.add('pe', mm, r=rkeys + tuple(("hTa", t) for t in range(t0, t1)), w=(("ps", bank),))
                evac(bank, t0, t1, T)

        for ci in range(4):
            k = load_slot(ci * 128)
            next_slot()
            proj_fm(k, (("slot", k, 0),), lambda bank, t0, t1, T, ci=ci: P.add(
                'act', lambda e: e.activation(out=qT[:, ci, t0 * 128:t0 * 128 + T], in_=ps[bank][:, :T], func=AF.Copy, scale=0.125),
                r=(("ps", bank),), w=(("qT", ci),)))
        for ci in range(4):
            k = load_slot(1536 + ci * 128)
            next_slot()
            proj_fm(k, (("slot", k, 0),), lambda bank, t0, t1, T, ci=ci: P.add(
                'act', lambda e: e.activation(out=iqT[:, ci, t0 * 128:t0 * 128 + T], in_=ps[bank][:, :T], func=AF.Copy),
                r=(("ps", bank),), w=tuple(("iqT", t) for t in range(t0, t1))))
        for ci in range(4):
            k = load_slot(512 + ci * 128)
            next_slot()
            def ev(bank, t0, t1, T, ci=ci):
                if t0 == 0:
                    return
                P.add('act', lambda e: e.activation(out=kT[:, ci, 2048 + (t0 - 1) * 128:2048 + (t0 - 1) * 128 + T], in_=ps[bank][:, :T], func=AF.Copy),
                      r=(("ps", bank),), w=(("kThi", ci),))
            proj_fm(k, (("slot", k, 0),), ev)
        k = load_slot(2048, 64, 0)
        load_slot(2048, 64, 64)
        next_slot()
        def ev(bank, t0, t1, T):
            if t0 == 0:
                return
            P.add('act', lambda e: e.activation(out=ik2T[:, 2048 + (t0 - 1) * 128:2048 + (t0 - 1) * 128 + T], in_=ps[bank][:, :T], func=AF.Copy),
                  r=(("ps", bank),), w=("ikhi",))
        proj_fm(k, (("slot", k, 0), ("slot", k, 64)), ev)
        for t in range(NT):
            if t >= 1:
                def mm(e, t=t):
                    for kc in range(8):
                        ins = e.matmul(ps[2], hT_all[:, kc, t * 128:(t + 1) * 128], Wv_[:, kc, :], start=(kc == 0), stop=(kc == 7))
                    return ins
                P.add('pe', mm, r=("mWv", ("hTa", t)), w=(("ps", 2),))
                P.add('act', lambda e, t=t: e.activation(out=V[:, 15 + t, :, 0:64], in_=ps[2].rearrange("p (h d) -> p h d", h=8), func=AF.Copy),
                      r=(("ps", 2), "V"), w=(("Vt", 15 + t),))
            def mm(e, t=t):
                for kc in range(8):
                    ins = e.matmul(ps[3][:, 0:8], hT_all[:, kc, t * 128:(t + 1) * 128], Wiw[:, kc, :], start=(kc == 0), stop=(kc == 7))
                return ins
            P.add('pe', mm, r=("mWiw", ("hTa", t)), w=(("ps", 3),))
            P.add('dve', lambda e, t=t: e.tensor_scalar(out=iw_sb[:, t, :], in0=ps[3][:, 0:8], scalar1=float(512 ** -0.5), scalar2=None, op0=ALU.mult),
                  r=(("ps", 3),), w=(("iw", t),))
        P.add('dve', lambda e: e.memset(pbuf[:, 0:2], 0.0), w=("pbuf",))
        CWK = tuple(("cw", i) for i in range(12))
        for cc in range(4):
            ku = load_slot(2120 + cc * 128)
            next_slot()
            kgc = load_slot(3144 + cc * 128)
            next_slot()
            kgb = load_slot(2632 + cc * 128)
            next_slot()
            for (t0, t1) in GROUPS17:
                T = (t1 - t0) * 128
                a0 = t0 * 128
                for (kk, bank) in ((ku, 4), (kgc, 5), (kgb, 6)):
                    def mm(e, kk=kk, bank=bank, a0=a0, T=T):
                        for kc in range(8):
                            ins = e.matmul(ps[bank][:, :T], slots[kk][:, kc, :], hT_all[:, kc, a0:a0 + T], start=(kc == 0), stop=(kc == 7))
                        return ins
                    P.add('pe', mm, r=(("slot", kk, 0),) + tuple(("hTa", t) for t in range(t0, t1)), w=(("ps", bank),))
                P.add('act', lambda e, T=T: e.activation(out=ucopy[:, :T], in_=ps[4][:, :T], func=AF.Copy), r=(("ps", 4),), w=("ucopy",))
                P.add('dve', lambda e, T=T, a0=a0: e.tensor_tensor(out=pbuf[:, 2 + a0:2 + a0 + T], in0=ps[5][:, :T], in1=ucopy[:, :T], op=ALU.mult),
                      r=(("ps", 5), "ucopy"), w=("pbuf",))
                P.add('dve', lambda e, T=T, a0=a0, cc=cc: e.tensor_scalar(out=ybuf[:, :T], in0=pbuf[:, 2 + a0:2 + a0 + T], scalar1=cw[:, cc, 2:3], scalar2=None, op0=ALU.mult),
                      r=("pbuf",) + CWK, w=("ybuf",))
                P.add('dve', lambda e, T=T, a0=a0, cc=cc: e.scalar_tensor_tensor(out=ybuf[:, :T], in0=pbuf[:, 1 + a0:1 + a0 + T], scalar=cw[:, cc, 1:2], in1=ybuf[:, :T],
                                                                               op0=ALU.mult, op1=ALU.add), r=("pbuf", "ybuf"), w=("ybuf",))
                P.add('dve', lambda e, T=T, a0=a0, cc=cc: e.scalar_tensor_tensor(out=ybuf[:, :T], in0=pbuf[:, a0:a0 + T], scalar=cw[:, cc, 0:1], in1=ybuf[:, :T],
                                                                               op0=ALU.mult, op1=ALU.add), r=("pbuf", "ybuf"), w=("ybuf",))
                P.add('dve', lambda e, T=T, a0=a0, cc=cc: e.tensor_tensor(out=cT[:, cc, a0:a0 + T], in0=ps[6][:, :T], in1=ybuf[:, :T], op=ALU.mult),
                      r=(("ps", 6), "ybuf"), w=(("cT", cc),))
        P.barrier()
        if SUBSTOP == "m2":
            raise _Stop()
        aS = Arena(nc, S0, S0 + 29728)
        biasN = aS.alloc([128, 8, 256], F32)
        r_ = [aS.alloc([128, 512], F32) for _ in range(2)]
        PT = [aS.alloc([128, 1024], BF16) for _ in range(2)]
        tmpb = aS.alloc([128, 1024], F32)
        a_tm = aS.alloc([128, 512], BF16)
        m2_off = aS.top
        maskT2 = aS.alloc([128, 32, 128], BF16)
        aM = Arena(nc, m2_off, m2_off + 8192)
        rb_sb = aM.alloc([32, 8], F32)
        oh_sb = aM.alloc([32, 383], F32)
        tv_sb = aM.alloc([8, 383], F32)
        score = aH.alloc([128, 4096], F32)
        mask = aH.alloc([128, 4096], BF16)
        maskT1 = aH.alloc([128, 32, 128], BF16)
        maskTs = [maskT1, maskT2]
        P.add('sp', lambda e: e.dma_start(out=rb_sb, in_=rel_bias), r=(), w=("rb_sb",), dma=True)
        P.add('sp', lambda e: e.dma_start(out=oh_sb, in_=c_onehot), r=(), w=("oh_sb",), dma=True)
        P.add('pe', lambda e: e.matmul(ps[0][0:8, 0:383], rb_sb, oh_sb, start=True, stop=True), r=("rb_sb", "oh_sb"), w=(("ps", 0),))
        P.add('act', lambda e: e.activation(out=tv_sb, in_=ps[0][0:8, 0:383], func=AF.Copy), r=(("ps", 0),), w=("tv_sb",))
        P.add('sp', lambda e: e.dma_start(out=tv_d, in_=tv_sb), r=("tv_sb",), w=("tv_d",), dma=True)
        for s_ in range(128):
            P.add('sp', lambda e, s_=s_: e.dma_start(out=biasN[s_:s_ + 1, :, :], in_=tv_d[:, 127 - s_:127 - s_ + 256].unsqueeze(0)),
                  r=("tv_d",), w=(("biasN_ld", s_),), dma=True, group="toe")
        for h in range(8):
            P.add('dve', lambda e, h=h: e.tensor_scalar(out=biasN[:, h, :], in0=biasN[:, h, :], scalar1=cfar[:, h:h + 1], scalar2=None, op0=ALU.subtract),
                  r=tuple(("biasN_ld", s_) for s_ in range(128)) + ("cfar",), w=(("biasN", h),))
        BIASK = tuple(("biasN", h) for h in range(8))
        if SUBSTOP == "m3":
            P.barrier()
            raise _Stop()
        ptc = [0]

        def EO(h):
            return (h % 2) * 512 + (h // 2) * 128

        def stageA1(lt):
            vt = 15 + lt
            nk = vt + 1
            N = nk * 128
            q0 = lt * 128
            nch = (N + 511) // 512
            for ch in range(nch):
                k0 = ch * 512
                Kw = min(512, N - k0)
                for h in range(8):
                    hp = (h % 2) * 64
                    hb = h % 2
                    P.add('pe', lambda e, h=h, hp=hp, hb=hb, k0=k0, Kw=Kw: e.matmul(ps[hb][:, :Kw], iqT[hp:hp + 64, h // 2, q0:q0 + 128], ik2T[hp:hp + 64, k0:k0 + Kw],
                                                                                 start=True, stop=True),
                          r=(("iqT", lt), "iklo", "ikhi"), w=(("ps", hb),))
                    P.add('act', lambda e, hb=hb, Kw=Kw: e.activation(out=r_[hb][:, :Kw], in_=ps[hb][:, :Kw], func=AF.Relu), r=(("ps", hb),), w=(("r_", hb),))
                    if h == 0:
                        P.add('dve', lambda e, hb=hb, k0=k0, Kw=Kw: e.tensor_scalar(out=score[:, k0:k0 + Kw], in0=r_[hb][:, :Kw], scalar1=iw_sb[:, lt, 0:1], scalar2=None, op0=ALU.mult),
                              r=(("r_", hb), ("iw", lt)), w=("score",))
                    else:
                        P.add('dve', lambda e, hb=hb, k0=k0, Kw=Kw, h=h: e.scalar_tensor_tensor(out=score[:, k0:k0 + Kw], in0=r_[hb][:, :Kw], scalar=iw_sb[:, lt, h:h + 1],
                                                                                           in1=score[:, k0:k0 + Kw], op0=ALU.mult, op1=ALU.add),
                              r=(("r_", hb), ("iw", lt), "score"), w=("score",))
            P.add('dve', lambda e: e.tensor_reduce(out=mx_, in_=score[:, :N], axis=AX.X, op=ALU.max), r=("score",), w=("mx",))
            P.add('dve', lambda e: e.tensor_reduce(out=mn_, in_=score[:, :N], axis=AX.X, op=ALU.min), r=("score",), w=("mn",))
            for ch in range(4):
                P.add('pe', lambda e, ch=ch: e.matmul(ps[2], onesb[0:1, :], kbrow[0:1, ch * 512:(ch + 1) * 512], start=True, stop=True),
                      r=("onesb", "kbrow"), w=(("ps", 2),))
                P.add('dve', lambda e, ch=ch: e.tensor_tensor(out=score[:, ch * 512:(ch + 1) * 512], in0=ps[2], in1=score[:, ch * 512:(ch + 1) * 512], op=ALU.add),
                      r=(("ps", 2), "score"), w=("score",))
            P.add('dve', lambda e: e.tensor_tensor(out=score[:, vt * 128:(vt + 1) * 128], in0=score[:, vt * 128:(vt + 1) * 128], in1=trif, op=ALU.add),
                  r=("score", "trif"), w=("score",))
            P.add('dve', lambda e: e.tensor_tensor(out=w0_, in0=mx_, in1=mn_, op=ALU.subtract), r=("mx", "mn"), w=("w0",))
            P.add('dve', lambda e: e.tensor_scalar(out=w0_, in0=w0_, scalar1=2.0, scalar2=None, op0=ALU.add), r=("w0",), w=("w0",))
            P.add('dve', lambda e: e.tensor_scalar(out=lo_, in0=mn_, scalar1=-1.0, scalar2=None, op0=ALU.add), r=("mn",), w=("lo",))
            P.add('dve', lambda e: e.tensor_scalar(out=wtab, in0=cjtab, scalar1=w0_[:, 0:1], scalar2=None, op0=ALU.mult), r=("w0", "cjtab"), w=("wtab",))
            for j in range(NBIS):
                P.add('dve', lambda e, j=j: e.tensor_tensor(out=mid_, in0=lo_, in1=wtab[:, j:j + 1], op=ALU.add), r=("lo", "wtab"), w=("mid",))
                P.add('dve', lambda e: e.tensor_scalar(out=mask[:, :N], in0=score[:, :N], scalar1=mid_[:, 0:1], scalar2=None, op0=ALU.is_ge, op1=ALU.add, accum_out=cnt_),
                      r=("score", "mid"), w=("mask", "cnt"))
                P.add('dve', lambda e, j=j: e.tensor_scalar(out=step_, in0=cnt_, scalar1=256.0, scalar2=wtab[:, j:j + 1], op0=ALU.is_ge, op1=ALU.mult),
                      r=("cnt", "wtab"), w=("step",))
                P.add('dve', lambda e: e.tensor_tensor(out=lo_, in0=lo_, in1=step_, op=ALU.add), r=("lo", "step"), w=("lo",))
            P.add('dve', lambda e: e.tensor_scalar(out=mask[:, :N], in0=score[:, :N], scalar1=lo_[:, 0:1], scalar2=-30000.0, op0=ALU.is_lt, op1=ALU.mult),
                  r=("score", "lo"), w=("mask",))

        def stageA2(lt):
            vt = 15 + lt
            nk = vt + 1
            mT = maskTs[lt % 2]
            for jb in range(0, nk, 8):
                n8 = min(8, nk - jb)
                pb3 = psbf(3)
                def tp(e, jb=jb, n8=n8, pb3=pb3):
                    for i in range(n8):
                        ins = e.transpose(pb3[:, i * 128:(i + 1) * 128], mask[:, (jb + i) * 128:(jb + i + 1) * 128], identb)
                    return ins
                P.add('pe', tp, r=("mask", "identb"), w=(("ps", 3),))
                P.add('act', lambda e, jb=jb, n8=n8, pb3=pb3: e.activation(out=mT[:, jb:jb + n8, :].rearrange("p a b -> p (a b)"), in_=pb3[:, :n8 * 128], func=AF.Copy),
                      r=(("ps", 3),), w=(("maskT", lt % 2),))

        def stageB(lt):
            vt = 15 + lt
            nk = vt + 1
            q0 = lt * 128
            mT = maskTs[lt % 2]
            for j in range(nk):
                near = (vt - j) <= 1
                off = (vt - j) * 128
                for h in range(8):
                    hp = (h % 2) * 64
                    bank = 4 + h % 2
                    def qk(e, h=h, hp=hp, bank=bank, j=j):
                        c0 = (h // 2) * 128
                        e.matmul(ps[bank][:, c0:c0 + 128], kT[hp:hp + 64, h // 2, j * 128:(j + 1) * 128], qT[hp:hp + 64, h // 2, q0:q0 + 128],
                                 start=True, stop=False, skip_group_check=True)
                        return e.matmul(ps[bank][:, c0:c0 + 128], identb, mT[:, j, :], start=False, stop=True, skip_group_check=True)
                    P.add('pe', qk, r=("kTlo", ("kThi", h // 2), ("qT", h // 2), ("maskT", lt % 2), "identb"), w=(("ps", bank),))
                pb = ptc[0] % 2
                ptc[0] += 1
                if near:
                    for h in range(8):
                        bank = 4 + h % 2
                        P.add('dve', lambda e, h=h, bank=bank, off=off: e.tensor_tensor(out=tmpb[:, EO(h):EO(h) + 128], in0=ps[bank][:, (h // 2) * 128:(h // 2 + 1) * 128],
                                                                                   in1=biasN[:, h, off:off + 128], op=ALU.add),
                              r=(("ps", bank),) + BIASK, w=("tmpb",))
                    P.add('act', lambda e, pb=pb: e.activation(out=PT[pb], in_=tmpb, func=AF.Exp), r=("tmpb",), w=(("PT", pb),))
                else:
                    for hh in range(2):
                        P.add('act', lambda e, hh=hh, pb=pb: e.activation(out=PT[pb][:, hh * 512:(hh + 1) * 512], in_=ps[4 + hh], func=AF.Exp),
                              r=(("ps", 4 + hh),), w=(("PT", pb),))
                def pv(e, pb=pb, j=j):
                    for h in range(8):
                        ins = e.matmul(ps[6 + h // 4][:, (h % 4) * 66:(h % 4) * 66 + 66], PT[pb][:, EO(h):EO(h) + 128], V[:, j, h, :],
                                       start=(j == 0 and h % 4 == 0), stop=(j == nk - 1), skip_group_check=True)
                    return ins
                P.add('pe', pv, r=(("PT", pb), ("Vt", j), "V"), w=(("ps", 6), ("ps", 7)))

        def stageF(lt):
            q0 = lt * 128
            for hh in range(2):
                P.add('dve', lambda e, hh=hh: e.tensor_scalar(out=den[:, hh * 4:(hh + 1) * 4], in0=ps[6 + hh][:, 0:264].rearrange("p (h c) -> p h c", c=66)[:, :, 64],
                                                             scalar1=1e-30, scalar2=None, op0=ALU.add), r=(("ps", 6 + hh),), w=(("den", hh),))
            P.add('dve', lambda e: e.reciprocal(out=rinv, in_=den), r=(("den", 0), ("den", 1)), w=("rinv",))
            for h in range(8):
                P.add('dve', lambda e, h=h: e.tensor_scalar(out=a_tm[:, h * 64:(h + 1) * 64], in0=ps[6 + h // 4][:, (h % 4) * 66:(h % 4) * 66 + 64], scalar1=rinv[:, h:h + 1],
                                                           scalar2=None, op0=ALU.mult), r=(("ps", 6 + h // 4), "rinv"), w=("a_tm",))
            pb3 = psbf(3)
            def tp(e, pb3=pb3):
                for ci in range(4):
                    ins = e.transpose(pb3[:, ci * 128:(ci + 1) * 128], a_tm[:, ci * 128:(ci + 1) * 128], identb)
                return ins
            P.add('pe', tp, r=("a_tm", "identb"), w=(("ps", 3),))
            for ci in range(4):
                P.add('act', lambda e, ci=ci, pb3=pb3: e.activation(out=aT[:, ci, q0:q0 + 128], in_=pb3[:, ci * 128:(ci + 1) * 128], func=AF.Copy),
                      r=(("ps", 3),), w=(("iqT", lt),))

        stageA1(0)
        stageA2(0)
        for lt_ in range(NT):
            if lt_ + 1 < NT:
                stageA1(lt_ + 1)
            stageB(lt_)
            if lt_ + 1 < NT:
                stageA2(lt_ + 1)
            stageF(lt_)
        P.barrier()
        aS = Arena(nc, S0, S0 + 29728)
        Wout = aS.alloc([128, 8, D], BF16)
        ns = alloc_ns(aS, nt=1)
        P.add('sp', lambda e: e.dma_start(out=Wout, in_=wout_bf.rearrange("(kc p) d -> p kc d", p=128)), r=(("wout",),), w=("Wout",), dma=True)
        load_gpost(mix_post[0])
        for t in range(NT):
            P.add('sp', lambda e, t=t: e.dma_start(out=X[:, t, :], in_=xspill[t]), r=(("xsp", t),), w=(("X", t),), dma=True, group="xrel0")
        for t in range(NT):
            for dh in range(2):
                def mm(e, t=t, dh=dh):
                    for kc in range(8):
                        src = aT if kc < 4 else cT
                        ins = e.matmul(ps[dh], src[:, kc % 4, t * 128:(t + 1) * 128], Wout[:, kc, dh * 512:(dh + 1) * 512], start=(kc == 0), stop=(kc == 7))
                    return ins
                P.add('pe', mm, r=(("iqT", t), "Wout") + tuple(("cT", c) for c in range(4)), w=(("ps", dh),))
            epilogue(ns, [ps[0], ps[1]], [("ps", 0), ("ps", 1)], t, 1.0)
        P.barrier()

    def load_own():
        for t in range(16):
            P.add('sp', lambda e, t=t: e.dma_start(out=X[:, 1 + t, :], in_=xo_v[:, t, :]), r=(), w=(("X", 1 + t),), dma=True, group="xown")

    def program():
        if STOP == "consts":
            load_own()
            return
        if STOP != "ffnown":
            PG, pre_load, after, a_holder = prev_ffn()
            pre_load(0)
            ffn_phase(0, 0, PG, after_group=after)
            P.barrier()
        load_own()
        ffn_phase(0, 0, GROUPS16)
        P.barrier()
        if STOP in ("ffn00", "ffnown"):
            return
        if STOP == "xa_only":
            xattn_phase(0, GROUPS17)
            P.barrier()
            return
        if STOP == "pool_only":
            pool_phase()
            P.barrier()
            return
        mixer0_phase()
        if STOP == "mix0":
            return
        xattn_phase(0, GROUPS17)
        P.barrier()
        if STOP == "xa0":
            return
        ffn_phase(0, 1, GROUPS17)
        P.barrier()
        if STOP == "ffn01":
            return
        ffn_phase(1, 0, GROUPS17)
        P.barrier()
        if STOP == "ffn10":
            return
        pool_phase()
        P.barrier()
        if STOP == "mix1":
            return
        xattn_phase(1, GROUPS16)
        P.barrier()
        if STOP == "xa1":
            return
        ffn_phase(1, 1, GROUPS16)
        P.barrier()

    try:
        program()
    except _Stop:
        P.barrier()

    for t in range(16):
        P.add('sp', lambda e, t=t: e.dma_start(out=out[t * 128:(t + 1) * 128, :], in_=X[:, 1 + t, :]),
              r=(("X", 1 + t),), w=(("out", t),), dma=True, group="outst")
    P.barrier()
    P.emit()
    return nc


def rest_of_program(env):
    return True


_CACHE = {}


def _consts(half):
    c = {}
    c["c_ident"] = np.eye(128, dtype=np.float32)
    q = np.arange(128)[:, None]
    s = np.arange(128)[None, :]
    c["c_tri"] = np.where(s > q, np.float32(-1e30), np.float32(0)).astype(np.float32)
    c["c_kb"] = np.full((1, 2048), 0.0 if half == 1 else -1e30, dtype=np.float32)
    e = np.arange(383) - 127
    b = t5_bucket_np(e)
    oh = np.zeros((32, 383), np.float32)
    oh[b, np.arange(383)] = 1.0
    c["c_onehot"] = oh
    c["c_halo"] = np.full((128, 1), float(half), dtype=np.float32)
    corr = np.ones((4, 16), np.float32)
    if half == 0:
        for g, win in enumerate((2, 4, 8, 16)):
            for t in range(16):
                corr[g, t] = win / min(t + 1, win)
    c["c_corr"] = np.broadcast_to(corr.reshape(1, 64), (128, 64)).copy()
    c["c_cj"] = np.broadcast_to((2.0 ** -(np.arange(NBIS) + 1.0)).astype(np.float32).reshape(1, NBIS), (128, NBIS)).copy()
    return c


WEIGHT_NAMES = ["ffn_w_gate", "ffn_w_up", "ffn_w_down", "ffn_norm_pre", "ffn_norm_post", "mix_norm_pre", "mix_norm_post",
                "even_w_in", "even_conv_w", "even_w_out", "rel_bias", "pool_w", "pool_scale", "xattn_norm_pre",
                "xattn_mem_norm", "xattn_norm_post", "xattn_wq", "xattn_wk", "xattn_wv", "xattn_wo"]


def kernel(**inputs):
    if "nc" not in _CACHE:
        _CACHE["nc"] = build()
    nc = _CACHE["nc"]
    x = np.ascontiguousarray(np.asarray(inputs["x"], dtype=np.float32))
    mem = np.ascontiguousarray(np.asarray(inputs["mem"], dtype=np.float32))
    shared = {k: np.ascontiguousarray(np.asarray(inputs[k], dtype=np.float32)) for k in WEIGHT_NAMES}
    zeros = np.zeros((2048, D), np.float32)
    in_maps = []
    for core in range(8):
        b, half = core // 2, core % 2
        m = dict(shared)
        m["x_own"] = np.ascontiguousarray(x[b, half * 2048:(half + 1) * 2048])
        m["x_prev"] = np.ascontiguousarray(x[b, 0:2048]) if half == 1 else zeros
        m["mem"] = np.ascontiguousarray(mem[b])
        m.update(_consts(half))
        in_maps.append(m)
    res = run_bass_kernel_spmd(nc, in_maps, core_ids=list(range(8)))
    outp = np.empty((4, 4096, D), np.float32)
    for core in range(8):
        b, half = core // 2, core % 2
        outp[b, half * 2048:(half + 1) * 2048] = res.results[core]["out"]
    return outp
```
